# Optimizing a Trainium2 kernel written in Bass

```python
import jax
import jax.numpy as jnp
from jax import lax
import numpy as np

D_MODEL = 1024
BATCH = 1
SEQ = 16384
DEPTH = 4

GRID_W = 64
CTX_LEN = 256
N_MIXERS = 4
N_MOD = 6

FN_GROUPS = 4

HEAD_DIM = 64
FA_Q_HEADS = 16
FA_KV_HEADS = 4
WA_Q_HEADS = 16
WA_KV_HEADS = 2
WINDOW = 128
Q_BLOCK = 128
ROPE_THETA = 10000.0

GM_CHUNK = 128
GM_DFFN = 4 * D_MODEL
GM_GROUPS = 8

N_EXPERTS = 32
TOP_K = 4
EXPERT_FF = D_MODEL
SWIGLU_LIMIT = 7.0
SWIGLU_ALPHA = 1.702
MOE_BLOCK = 128

LN_EPS = 1e-5
RMS_EPS = 1e-6
NEG_INF = -1e30
DEEPNORM_ALPHA = (2 * DEPTH) ** 0.25
DEEPNORM_BETA = (8 * DEPTH) ** -0.25

N_FN = len(range(0, DEPTH, N_MIXERS))
N_FA = len(range(1, DEPTH, N_MIXERS))
N_GM = len(range(2, DEPTH, N_MIXERS))
N_WA = len(range(3, DEPTH, N_MIXERS))

kernel_name = 'hybrid_dit_fourier_gqa_gmlp_swa_moe'


def _layer_norm(x, g, b):
    xf = x.astype(jnp.float32)
    mu = jnp.mean(xf, axis=-1, keepdims=True)
    var = jnp.mean(jnp.square(xf - mu), axis=-1, keepdims=True)
    y = (xf - mu) * lax.rsqrt(var + LN_EPS) * g.astype(jnp.float32) + b.astype(jnp.float32)
    return y.astype(x.dtype)


def _rms_norm(x, g):
    xf = x.astype(jnp.float32)
    y = xf * lax.rsqrt(jnp.mean(xf * xf, axis=-1, keepdims=True) + RMS_EPS) * g.astype(jnp.float32)
    return y.astype(x.dtype)


def _modulate(x, shift, scale):
    return x * (1 + scale) + shift


def _axial_rope_tables(rows):
    row = jnp.repeat(jnp.arange(rows, dtype=jnp.float32), GRID_W)
    col = jnp.tile(jnp.arange(GRID_W, dtype=jnp.float32), rows)
    n_freq = HEAD_DIM // 4
    inv = ROPE_THETA ** (-jnp.arange(n_freq, dtype=jnp.float32) / n_freq)
    ang = jnp.concatenate([row[:, None] * inv, col[:, None] * inv], axis=-1)
    return jnp.cos(ang), jnp.sin(ang)


def _apply_rope(x, cos, sin):
    xf = x.astype(jnp.float32)
    x1, x2 = xf[..., 0::2], xf[..., 1::2]
    out = jnp.stack([x1 * cos - x2 * sin, x1 * sin + x2 * cos], axis=-1).reshape(xf.shape)
    return out.astype(x.dtype)


def _qkv(h, w_qkv, b_qkv, n_q, n_kv):
    bsz, n, _ = h.shape
    grp = n_q // n_kv
    qkv = h @ w_qkv + b_qkv
    q, k, v = jnp.split(qkv, [n_q * HEAD_DIM, (n_q + n_kv) * HEAD_DIM], axis=-1)
    q = q.reshape(bsz, n, n_kv, grp, HEAD_DIM).transpose(0, 2, 3, 1, 4)
    k = k.reshape(bsz, n, n_kv, HEAD_DIM).transpose(0, 2, 1, 3)
    v = v.reshape(bsz, n, n_kv, HEAD_DIM).transpose(0, 2, 1, 3)
    return q, k, v


def _merge_heads(o):
    bsz, hk, grp, n, dh = o.shape
    return o.transpose(0, 3, 1, 2, 4).reshape(bsz, n, hk * grp * dh)


def _gqa_softmax(q, k, v, mask=None, sink=None):
    s = jnp.einsum('bhgqd,bhkd->bhgqk', q, k, preferred_element_type=jnp.float32) * (HEAD_DIM ** -0.5)
    if mask is not None:
        s = jnp.where(mask, s, NEG_INF)
    if sink is not None:
        sk = jnp.broadcast_to(sink.astype(jnp.float32)[None, :, :, None, None], s.shape[:-1] + (1,))
        s = jnp.concatenate([s, sk], axis=-1)
    p = jax.nn.softmax(s, axis=-1)
    if sink is not None:
        p = p[..., :-1]
    return jnp.einsum('bhgqk,bhkd->bhgqd', p.astype(v.dtype), v)


def _fourier_mixer(h_lat, h_ctx, w_out, b_out, with_ctx):
    def run(h):
        bsz, n, d = h.shape
        hg = h.astype(jnp.float32).reshape(bsz, n, FN_GROUPS, d // FN_GROUPS)
        mixed = jnp.fft.fft2(hg, axes=(1, 3), norm='ortho').real.astype(h.dtype).reshape(bsz, n, d)
        return mixed @ w_out + b_out
    return run(h_lat), (run(h_ctx) if with_ctx else None)


def _full_attention_mixer(h_lat, h_ctx, cos, sin, w_qkv, b_qkv, q_norm, k_norm, w_out, b_out, with_ctx):
    bsz, n, _ = h_lat.shape
    grp = FA_Q_HEADS // FA_KV_HEADS
    nb = n // Q_BLOCK
    q, k, v = _qkv(h_lat, w_qkv, b_qkv, FA_Q_HEADS, FA_KV_HEADS)
    q = _apply_rope(_rms_norm(q, q_norm), cos, sin)
    k = _apply_rope(_rms_norm(k, k_norm), cos, sin)
    qc, kc, vc = _qkv(h_ctx, w_qkv, b_qkv, FA_Q_HEADS, FA_KV_HEADS)
    qc = _rms_norm(qc, q_norm)
    kc = _rms_norm(kc, k_norm)
    k_all = jnp.concatenate([k, kc], axis=2)
    v_all = jnp.concatenate([v, vc], axis=2)
    q_blocks = jnp.moveaxis(q.reshape(bsz, FA_KV_HEADS, grp, nb, Q_BLOCK, HEAD_DIM), 3, 0)
    o = lax.map(lambda qb: _gqa_softmax(qb, k_all, v_all), q_blocks)
    o = jnp.moveaxis(o, 0, 3).reshape(bsz, FA_KV_HEADS, grp, n, HEAD_DIM)
    y_lat = _merge_heads(o) @ w_out + b_out
    y_ctx = (_merge_heads(_gqa_softmax(qc, kc, vc)) @ w_out + b_out) if with_ctx else None
    return y_lat, y_ctx


def _gmlp_mixer(h_lat, h_ctx, w_in, b_in, v_norm_g, v_norm_b, w_s, b_s, w_out, b_out, with_ctx):
    half = GM_DFFN // 2
    def run(h):
        bsz, n, _ = h.shape
        z = jax.nn.gelu(h @ w_in + b_in, approximate=False)
        u, v = jnp.split(z, 2, axis=-1)
        v = _layer_norm(v, v_norm_g, v_norm_b)
        v = v.reshape(bsz, n // GM_CHUNK, GM_CHUNK, GM_GROUPS, half // GM_GROUPS)
        v = jnp.einsum('hpq,bnqhc->bnphc', w_s, v) + b_s.T[None, None, :, :, None]
        return (u * v.reshape(bsz, n, half)) @ w_out + b_out
    return run(h_lat), (run(h_ctx) if with_ctx else None)


def _window_attention_mixer(h_lat, h_ctx, cos, sin, w_qkv, b_qkv, sink, w_out, b_out, with_ctx):
    bsz, n, _ = h_lat.shape
    grp = WA_Q_HEADS // WA_KV_HEADS
    nb = n // Q_BLOCK
    span = Q_BLOCK + 2 * WINDOW
    q, k, v = _qkv(h_lat, w_qkv, b_qkv, WA_Q_HEADS, WA_KV_HEADS)
    q = _apply_rope(q, cos, sin)
    k = _apply_rope(k, cos, sin)
    qc, kc, vc = _qkv(h_ctx, w_qkv, b_qkv, WA_Q_HEADS, WA_KV_HEADS)
    sink_hg = sink.reshape(WA_KV_HEADS, grp)
    pad = ((0, 0), (0, 0), (WINDOW, WINDOW), (0, 0))
    k_pad = jnp.pad(k, pad)
    v_pad = jnp.pad(v, pad)
    qi = jnp.arange(Q_BLOCK)[:, None]
    kj = jnp.arange(span)[None, :]
    band = jnp.abs(kj - WINDOW - qi) <= WINDOW
    ctx_ok = jnp.ones((Q_BLOCK, kc.shape[2]), dtype=bool)
    q_blocks = jnp.moveaxis(q.reshape(bsz, WA_KV_HEADS, grp, nb, Q_BLOCK, HEAD_DIM), 3, 0)

    def block(args):
        qb, b_idx = args
        start = b_idx * Q_BLOCK
        kw = lax.dynamic_slice_in_dim(k_pad, start, span, axis=2)
        vw = lax.dynamic_slice_in_dim(v_pad, start, span, axis=2)
        kpos = start - WINDOW + jnp.arange(span)
        valid = band & ((kpos >= 0) & (kpos < n))[None, :]
        mask = jnp.concatenate([valid, ctx_ok], axis=-1)
        return _gqa_softmax(qb, jnp.concatenate([kw, kc], axis=2), jnp.concatenate([vw, vc], axis=2), mask, sink_hg)

    o = lax.map(block, (q_blocks, jnp.arange(nb)))
    o = jnp.moveaxis(o, 0, 3).reshape(bsz, WA_KV_HEADS, grp, n, HEAD_DIM)
    y_lat = _merge_heads(o) @ w_out + b_out
    y_ctx = (_merge_heads(_gqa_softmax(qc, kc, vc, None, sink_hg)) @ w_out + b_out) if with_ctx else None
    return y_lat, y_ctx


def _moe(h, router_w, router_b, w_gate_up, b_gate_up, w_down, b_down):
    n_tok, d = h.shape
    logits = (h @ router_w + router_b).astype(jnp.float32)
    top_val, top_idx = lax.top_k(logits, TOP_K)
    gates = jax.nn.softmax(top_val, axis=-1)
    n_assign = n_tok * TOP_K
    n_blocks = -(-n_assign // MOE_BLOCK) + N_EXPERTS
    n_slots = n_blocks * MOE_BLOCK
    flat_e = top_idx.reshape(-1)
    flat_tok = jnp.repeat(jnp.arange(n_tok, dtype=jnp.int32), TOP_K)
    flat_g = gates.reshape(-1)
    order = jnp.argsort(flat_e, stable=True)
    e_sorted = flat_e[order]
    counts = jnp.bincount(flat_e, length=N_EXPERTS)
    padded = (counts + MOE_BLOCK - 1) // MOE_BLOCK * MOE_BLOCK
    ends_pad = jnp.cumsum(padded)
    starts = jnp.cumsum(counts) - counts
    dest = (ends_pad - padded)[e_sorted] + jnp.arange(n_assign) - starts[e_sorted]
    slot_tok = jnp.full((n_slots,), n_tok, dtype=jnp.int32).at[dest].set(flat_tok[order])
    slot_gate = jnp.zeros((n_slots,), dtype=jnp.float32).at[dest].set(flat_g[order])
    block_expert = jnp.minimum(jnp.searchsorted(ends_pad, jnp.arange(n_blocks) * MOE_BLOCK, side='right'), N_EXPERTS - 1)
    h_pad = jnp.concatenate([h, jnp.zeros((1, d), h.dtype)], axis=0)
    xb = h_pad[slot_tok].reshape(n_blocks, MOE_BLOCK, d)

    def expert_block(args):
        xe, e = args
        gu = xe @ w_gate_up[e] + b_gate_up[e]
        x_glu = jnp.minimum(gu[:, 0::2], SWIGLU_LIMIT)
        x_lin = jnp.clip(gu[:, 1::2], -SWIGLU_LIMIT, SWIGLU_LIMIT)
        act = x_glu * jax.nn.sigmoid(SWIGLU_ALPHA * x_glu) * (x_lin + 1)
        return act @ w_down[e] + b_down[e]

    yb = lax.map(expert_block, (xb, block_expert)).reshape(n_slots, d)
    y = jax.ops.segment_sum(yb * slot_gate[:, None].astype(yb.dtype), slot_tok, num_segments=n_tok + 1)
    return y[:n_tok]


def setup_inputs(seed: int = 0) -> dict:
    key = jax.random.key(seed)
    keys = iter(jax.random.split(key, 48))

    def nrm(shape, scale):
        return jax.random.normal(next(keys), shape, jnp.float32) * scale

    d = D_MODEL
    fa_qkv = (FA_Q_HEADS + 2 * FA_KV_HEADS) * HEAD_DIM
    wa_qkv = (WA_Q_HEADS + 2 * WA_KV_HEADS) * HEAD_DIM
    gm_half = GM_DFFN // 2
    return {
        'x': nrm((BATCH, SEQ, d), 1.0),
        'c': nrm((BATCH, d), 1.0),
        'ctx': nrm((BATCH, CTX_LEN, d), 1.0),
        'c_ctx': nrm((d,), 1.0),
        'ada_w': nrm((DEPTH, d, N_MOD * d), d ** -0.5),
        'ada_b': nrm((DEPTH, N_MOD * d), 0.02),
        'ln_mix_g': 1.0 + nrm((DEPTH, d), 0.02),
        'ln_mix_b': nrm((DEPTH, d), 0.02),
        'ln_ffn_g': 1.0 + nrm((DEPTH, d), 0.02),
        'ln_ffn_b': nrm((DEPTH, d), 0.02),
        'fn_w_out': nrm((N_FN, d, d), DEEPNORM_BETA * d ** -0.5),
        'fn_b_out': nrm((N_FN, d), 0.02),
        'fa_w_qkv': nrm((N_FA, d, fa_qkv), d ** -0.5),
        'fa_b_qkv': nrm((N_FA, fa_qkv), 0.02),
        'fa_q_norm': 1.0 + nrm((N_FA, HEAD_DIM), 0.02),
        'fa_k_norm': 1.0 + nrm((N_FA, HEAD_DIM), 0.02),
        'fa_w_out': nrm((N_FA, FA_Q_HEADS * HEAD_DIM, d), DEEPNORM_BETA * (FA_Q_HEADS * HEAD_DIM) ** -0.5),
        'fa_b_out': nrm((N_FA, d), 0.02),
        'gm_w_in': nrm((N_GM, d, GM_DFFN), d ** -0.5),
        'gm_b_in': nrm((N_GM, GM_DFFN), 0.02),
        'gm_v_norm_g': 1.0 + nrm((N_GM, gm_half), 0.02),
        'gm_v_norm_b': nrm((N_GM, gm_half), 0.02),
        'gm_w_s': nrm((N_GM, GM_GROUPS, GM_CHUNK, GM_CHUNK), GM_CHUNK ** -0.5),
        'gm_b_s': 1.0 + nrm((N_GM, GM_GROUPS, GM_CHUNK), 0.02),
        'gm_w_out': nrm((N_GM, gm_half, d), DEEPNORM_BETA * gm_half ** -0.5),
        'gm_b_out': nrm((N_GM, d), 0.02),
        'wa_w_qkv': nrm((N_WA, d, wa_qkv), d ** -0.5),
        'wa_b_qkv': nrm((N_WA, wa_qkv), 0.02),
        'wa_sink': nrm((N_WA, WA_Q_HEADS), 0.5),
        'wa_w_out': nrm((N_WA, WA_Q_HEADS * HEAD_DIM, d), DEEPNORM_BETA * (WA_Q_HEADS * HEAD_DIM) ** -0.5),
        'wa_b_out': nrm((N_WA, d), 0.02),
        'router_w': nrm((DEPTH, d, N_EXPERTS), d ** -0.5),
        'router_b': nrm((DEPTH, N_EXPERTS), 0.01),
        'exp_w_gate_up': nrm((DEPTH, N_EXPERTS, d, 2 * EXPERT_FF), d ** -0.5),
        'exp_b_gate_up': nrm((DEPTH, N_EXPERTS, 2 * EXPERT_FF), 0.02),
        'exp_w_down': nrm((DEPTH, N_EXPERTS, EXPERT_FF, d), DEEPNORM_BETA * EXPERT_FF ** -0.5),
        'exp_b_down': nrm((DEPTH, N_EXPERTS, d), 0.02),
    }


def reference(x, c, ctx, c_ctx, ada_w, ada_b, ln_mix_g, ln_mix_b, ln_ffn_g, ln_ffn_b,
              fn_w_out, fn_b_out,
              fa_w_qkv, fa_b_qkv, fa_q_norm, fa_k_norm, fa_w_out, fa_b_out,
              gm_w_in, gm_b_in, gm_v_norm_g, gm_v_norm_b, gm_w_s, gm_b_s, gm_w_out, gm_b_out,
              wa_w_qkv, wa_b_qkv, wa_sink, wa_w_out, wa_b_out,
              router_w, router_b, exp_w_gate_up, exp_b_gate_up, exp_w_down, exp_b_down):
    bsz, n_lat, d = x.shape
    n_ctx = ctx.shape[1]
    rows = n_lat // GRID_W
    cos, sin = _axial_rope_tables(rows)
    silu_c = jax.nn.silu(c)
    silu_cc = jax.nn.silu(c_ctx)
    for i in range(DEPTH):
        kind, j = i % N_MIXERS, i // N_MIXERS
        with_ctx = i < DEPTH - 1
        sh1, sc1, g1, sh2, sc2, g2 = jnp.split((silu_c @ ada_w[i] + ada_b[i])[:, None, :], N_MOD, axis=-1)
        sh1c, sc1c, g1c, sh2c, sc2c, g2c = jnp.split((silu_cc @ ada_w[i] + ada_b[i])[None, None, :], N_MOD, axis=-1)
        h_lat = _modulate(x, sh1, sc1)
        h_ctx = _modulate(ctx, sh1c, sc1c)
        if kind == 0:
            y_lat, y_ctx = _fourier_mixer(h_lat, h_ctx, fn_w_out[j], fn_b_out[j], with_ctx)
        elif kind == 1:
            y_lat, y_ctx = _full_attention_mixer(h_lat, h_ctx, cos, sin, fa_w_qkv[j], fa_b_qkv[j], fa_q_norm[j],
                                                 fa_k_norm[j], fa_w_out[j], fa_b_out[j], with_ctx)
        elif kind == 2:
            y_lat, y_ctx = _gmlp_mixer(h_lat, h_ctx, gm_w_in[j], gm_b_in[j], gm_v_norm_g[j], gm_v_norm_b[j],
                                       gm_w_s[j], gm_b_s[j], gm_w_out[j], gm_b_out[j], with_ctx)
        else:
            y_lat, y_ctx = _window_attention_mixer(h_lat, h_ctx, cos, sin, wa_w_qkv[j], wa_b_qkv[j], wa_sink[j],
                                                   wa_w_out[j], wa_b_out[j], with_ctx)
        x = _layer_norm(DEEPNORM_ALPHA * x + g1 * y_lat, ln_mix_g[i], ln_mix_b[i])
        h_lat = _modulate(x, sh2, sc2)
        moe_args = (router_w[i], router_b[i], exp_w_gate_up[i], exp_b_gate_up[i], exp_w_down[i], exp_b_down[i])
        if with_ctx:
            ctx = _layer_norm(DEEPNORM_ALPHA * ctx + g1c * y_ctx, ln_mix_g[i], ln_mix_b[i])
            h_ctx = _modulate(ctx, sh2c, sc2c)
            tok = jnp.concatenate([h_ctx, h_lat], axis=1).reshape(-1, d)
            f = _moe(tok, *moe_args).reshape(bsz, n_ctx + n_lat, d)
            f_ctx, f_lat = f[:, :n_ctx], f[:, n_ctx:]
            ctx = _layer_norm(DEEPNORM_ALPHA * ctx + g2c * f_ctx, ln_ffn_g[i], ln_ffn_b[i])
        else:
            f_lat = _moe(h_lat.reshape(-1, d), *moe_args).reshape(bsz, n_lat, d)
        x = _layer_norm(DEEPNORM_ALPHA * x + g2 * f_lat, ln_ffn_g[i], ln_ffn_b[i])
    return x
```

```python
import contextlib
import numpy as np
import concourse.bass as bass
import concourse.mybir as mybir
from concourse.bass_utils import run_bass_kernel_spmd

F32 = mybir.dt.float32
BF16 = mybir.dt.bfloat16
AF = mybir.ActivationFunctionType
ALU = mybir.AluOpType

ISSUERS = ("pe", "act", "dve", "pool", "sp")


class Op:
    __slots__ = ("eng", "fn", "is_dma", "waits", "signal", "sem", "semval", "idx")

    def __init__(self, eng, fn, is_dma):
        self.eng = eng
        self.fn = fn
        self.is_dma = is_dma
        self.waits = []
        self.signal = False
        self.sem = None
        self.semval = 0


class Prog:
    def __init__(self, nc, n_dma_sems=8):
        self.nc = nc
        self.streams = {e: [] for e in ISSUERS}
        self.last_w = {}
        self.readers = {}
        self.n_dma_sems = n_dma_sems
        self.final_ops = []
        self.fence = {}
        self.dma_ops = {e: [] for e in ISSUERS}

    def barrier(self):
        deps = []
        for e in ISSUERS:
            if self.streams[e]:
                deps.append(self.streams[e][-1])
            deps.extend(self.dma_ops[e][-self.n_dma_sems:])
        self.fence = {e: list(deps) for e in ISSUERS}

    def _add(self, eng, fn, reads, writes, is_dma):
        op = Op(eng, fn, is_dma)
        op.idx = len(self.streams[eng])
        deps = []
        for r in reads:
            w = self.last_w.get(r)
            if w is not None:
                deps.append(w)
        for wkey in writes:
            w = self.last_w.get(wkey)
            if w is not None:
                deps.append(w)
            deps.extend(self.readers.get(wkey, ()))
        fz = self.fence.pop(eng, None)
        if fz:
            deps.extend(fz)
        if is_dma:
            self.dma_ops[eng].append(op)
        seen = set()
        best = {}
        for d in deps:
            if id(d) in seen or d is op:
                continue
            seen.add(id(d))
            if d.is_dma:
                op.waits.append(d)
                continue
            if d.eng == eng == "pe" and not is_dma and not fz:
                continue
            b = best.get(d.eng)
            if b is None or d.idx > b.idx:
                best[d.eng] = d
        op.waits.extend(best.values())
        for r in reads:
            lst = self.readers.setdefault(r, [])
            if not is_dma:
                for i_, o_ in enumerate(lst):
                    if (not o_.is_dma) and o_.eng == eng:
                        lst[i_] = op
                        break
                else:
                    lst.append(op)
            else:
                lst.append(op)
        for wkey in writes:
            self.last_w[wkey] = op
            self.readers[wkey] = []
        self.streams[eng].append(op)
        return op

    def op(self, eng, fn, reads=(), writes=()):
        return self._add(eng, fn, reads, writes, False)

    def dma(self, eng, fn, reads=(), writes=(), final=False):
        op = self._add(eng, fn, reads, writes, True)
        if final:
            self.final_ops.append(op)
        return op

    def emit(self):
        nc = self.nc
        if self.final_ops:
            fin = Op("sp", None, False)
            fin.waits = list(self.final_ops)
            self.streams["sp"].append(fin)
        for e in ISSUERS:
            for op in self.streams[e]:
                for d in op.waits:
                    d.signal = True
        with contextlib.ExitStack() as st:
            csem = {e: st.enter_context(nc.semaphore(f"c_{e}")) for e in ISSUERS}
            dsem = {e: [st.enter_context(nc.semaphore(f"d_{e}{i}")) for i in range(self.n_dma_sems)]
                    for e in ("sp", "act", "pool")}
            extra = []
            for e in ISSUERS:
                cnt = 0
                dcnt = [0] * self.n_dma_sems
                rr = 0
                prev_on_sem = [None] * self.n_dma_sems
                for op in self.streams[e]:
                    if op.is_dma:
                        s = rr % self.n_dma_sems
                        rr += 1
                        dcnt[s] += 16
                        op.sem = dsem[e][s]
                        op.semval = dcnt[s]
                        p = prev_on_sem[s]
                        if p is not None:
                            op.waits.append(p)
                        prev_on_sem[s] = op
                    elif op.signal:
                        cnt += 1
                        if cnt > 12000:
                            cnt = 1
                            csem[e] = st.enter_context(nc.semaphore(f"c_{e}_{len(extra)}"))
                            extra.append(1)
                        op.sem = csem[e]
                        op.semval = cnt
            block = st.enter_context(nc.Block())
            engs = {"pe": block.tensor, "act": block.scalar, "dve": block.vector,
                    "pool": block.gpsimd, "sp": block.sync}

            def make(e):
                def body(eng):
                    known = {}
                    for op in self.streams[e]:
                        for d in op.waits:
                            k = id(d.sem)
                            if known.get(k, 0) >= d.semval:
                                continue
                            eng.wait_ge(d.sem, d.semval)
                            known[k] = d.semval
                        if op.fn is None:
                            continue
                        ins = op.fn(eng)
                        if op.is_dma:
                            ins.then_inc(op.sem, 16)
                        elif op.signal:
                            ins.then_inc(op.sem, 1)
                return body

            for e in ISSUERS:
                if self.streams[e]:
                    engs[e](make(e))


D = 1024
NLAT = 16384
NCTX = 256
NTOK = NLAT + NCTX
NSUB = NTOK // 128
DEPTH = 4
ALPHA = float((2 * DEPTH) ** 0.25)
LN_EPS = 1e-5
NE = 32


class StopBuild(Exception):
    pass


STOP = [99]


def checkpoint(n):
    return STOP[0] <= n


class Ring:
    def __init__(self, K, name, n, shape, dt, psum=False):
        self.t = [(K.ps if psum else K.sb)(f"{name}{i}", shape, dt) for i in range(n)]
        self.keys = [f"{name}{i}" for i in range(n)]
        self.i = -1

    def next(self):
        self.i = (self.i + 1) % len(self.t)
        return self.t[self.i], self.keys[self.i]


class K:
    def __init__(self, layers):
        self.layers = layers
        self.nc = bass.Bass("TRN2", target_bir_lowering=False)
        self.P = Prog(self.nc)
        self.root = contextlib.ExitStack()
        self.scope = self.root
        self.uid = 0
        self.inputs = {}

    def din(self, name, shape, dt=F32):
        if name in self.inputs:
            return self.inputs[name]
        ap = self.nc.dram_tensor(name, list(shape), dt, kind="ExternalInput").ap()
        self.inputs[name] = ap
        return ap

    def dscr(self, name, shape, dt=F32):
        return self.nc.dram_tensor(name, list(shape), dt, kind="Internal").ap()

    def sb(self, name, shape, dt=F32):
        self.uid += 1
        return self.scope.enter_context(self.nc.sbuf_tensor(f"{name}_{self.uid}", list(shape), dt))

    def ps(self, name, shape, dt=F32):
        self.uid += 1
        return self.scope.enter_context(self.nc.psum_tensor(f"{name}_{self.uid}", list(shape), dt))

    @contextlib.contextmanager
    def sub(self):
        old = self.scope
        with contextlib.ExitStack() as s:
            self.scope = s
            try:
                yield
            finally:
                self.scope = old
                self.P.barrier()

    def bank(self):
        return self.banks.next()

    def load(self, eng, out, in_, reads, writes):
        return self.P.dma(eng, lambda e: e.dma_start(out=out, in_=in_), reads=reads, writes=writes)

    def rowload(self, tile, key, vec_ap, rkey=None):
        n = vec_ap.shape[-1]
        self.P.dma("sp", lambda e: e.dma_start(out=tile, in_=vec_ap.to_broadcast([128, n])),
                   reads=[rkey] if rkey else [], writes=[key])


def build(layers, out_layer_dbg=False):
    k = K(layers)
    nc, P = k.nc, k.P
    x_in = k.din("x", [NLAT, D])
    ctx_in = k.din("ctx", [NCTX, D])
    cc_in = k.din("cvec", [2, D])
    ident_in = k.din("ident", [128, 128])
    out = nc.dram_tensor("out", [NLAT, D], F32, kind="ExternalOutput").ap()
    X = k.dscr("X", [NTOK, D])
    ROWS = k.dscr("ROWS", [12, 128, D])
    H2T = k.dscr("H2T", [NSUB, 128, 8, 128], BF16)
    GATES = k.dscr("GATES", [NSUB, 128, NE])

    with k.sub():
        k.banks = Ring(k, "bank", 5, [128, 512], F32, psum=True)
        k.accs = Ring(k, "accb", 2, [128, 512], F32, psum=True)
        tpb = k.ps("tpb", [128, 1024], BF16)
        ident = k.sb("ident", [128, 128], BF16)
        P.dma("pool", lambda e: e.dma_start(out=ident[:], in_=ident_in), writes=["ident"])
        ones32 = k.sb("ones32", [128, 128])
        P.op("pool", lambda e: e.memset(ones32[:], 1.0), writes=["ones32"])
        onesb = k.sb("onesb", [128, 128], BF16)
        P.op("pool", lambda e: e.memset(onesb[:], 1.0), writes=["onesb"])
        zeros = k.sb("zeros", [128, 128])
        P.op("pool", lambda e: e.memset(zeros[:], 0.0), writes=["zeros"])
        ident32 = k.sb("ident32", [128, 128])
        P.dma("sp", lambda e: e.dma_start(out=ident32[:], in_=ident_in), writes=["ident32"])
        gring = Ring(k, "gt", 2, [128, NE + 2], F32)
        epsc = k.sb("epsc", [128, 1])
        P.op("pool", lambda e: e.memset(epsc[:], LN_EPS), writes=["epsc"])
        for q in range(16):
            P.dma("sp", lambda e, q=q: e.dma_start(out=X[q * 1024:(q + 1) * 1024, :], in_=x_in[q * 1024:(q + 1) * 1024, :]), writes=[("Xinit", q)])
        P.op("sp", lambda e: e.nop(), reads=[("Xinit", q) for q in range(16)], writes=["X"])
        P.dma("sp", lambda e: e.dma_start(out=X[NLAT:NTOK, :], in_=ctx_in), writes=["Xc"])
        cT = k.sb("cT", [128, 2, 8])
        with nc.allow_non_contiguous_dma(reason="tiny"):
            P.dma("sp", lambda e: e.dma_start(out=cT[:], in_=cc_in.rearrange("w (kc p) -> p w kc", p=128), allow_slow_non_contiguous=True), writes=["cT"])
        cS = k.sb("cS", [128, 2, 8])
        P.op("act", lambda e: e.activation(out=cS[:], in_=cT[:], func=AF.Silu), reads=["cT"], writes=["cS"])
        LB = k.sb("LB", [128, 2, 8, 128])
        for w in range(2):
            for kc in range(8):
                P.op("act", lambda e, w=w, kc=kc: e.activation(out=LB[:, w, kc, :], in_=zeros[:], func=AF.Identity,
                                                           bias=cS[:, w, kc:kc + 1], scale=1.0),
                     reads=["cS", "zeros"], writes=[("LB", w, kc)])
        LBkeys = [("LB", w, kc) for w in range(2) for kc in range(8)]

        try:
          for li in layers:
            layer(k, li, dict(X=X, ROWS=ROWS, H2T=H2T, GATES=GATES, ident=ident, ones32=ones32, onesb=onesb,
                              zeros=zeros, epsc=epsc, ident32=ident32, gring=gring, LB=LB, LBkeys=LBkeys, tpb=tpb))
        except StopBuild:
            pass

        P.dma("sp", lambda e: e.dma_start(out=out, in_=X[0:NLAT, :]), reads=["X"], final=True)
        P.emit()
    return k


def ada_rows(k, li, C):
    nc, P = k.nc, k.P
    aw = k.din(f"ada_w{li}", [D, 6 * D])
    ab = k.din(f"ada_b{li}", [1, 6 * D])
    ROWS, LB, ones32 = C["ROWS"], C["LB"], C["ones32"]
    with k.sub():
        wr = Ring(k, "adaw", 2, [128, 8, 512], F32)
        br = Ring(k, "adab", 2, [1, 512], F32)
        orr = Ring(k, "adao", 3, [128, 512], F32)
        for ct in range(12):
            wt, wk = wr.next()
            bt, bk = br.next()
            P.dma("sp", lambda e, wt=wt, ct=ct: e.dma_start(
                out=wt[:], in_=aw[:, ct * 512:(ct + 1) * 512].rearrange("(kc p) f -> p kc f", p=128)), writes=[wk])
            P.dma("sp", lambda e, bt=bt, ct=ct: e.dma_start(out=bt[:], in_=ab[:, ct * 512:(ct + 1) * 512]), writes=[bk])
            j, half = ct // 2, ct % 2
            for w in range(2):
                pb, pk = k.bank()

                def mm(e, pb=pb, wt=wt, bt=bt, w=w):
                    for kc in range(8):
                        e.matmul(pb[:], lhsT=LB[:, w, kc, :], rhs=wt[:, kc, :], start=(kc == 0), stop=False)
                    return e.matmul(pb[:], lhsT=ones32[0:1, :], rhs=bt[0:1, :], start=False, stop=True)
                P.op("pe", mm, reads=[wk, bk, "ones32"] + C["LBkeys"], writes=[pk])
                ot, ok = orr.next()
                add = 1.0 if j in (1, 4) else 0.0
                P.op("dve", lambda e, ot=ot, pb=pb, add=add: e.tensor_scalar(
                    out=ot[:], in0=pb[:], scalar1=add, scalar2=None, op0=ALU.add), reads=[pk], writes=[ok])
                P.dma("sp", lambda e, ot=ot, w=w, j=j, half=half: e.dma_start(
                    out=ROWS[w * 6 + j, :, half * 512:(half + 1) * 512], in_=ot[:]), reads=[ok], writes=[("ROWS", w * 6 + j, half)])


def rows_keys(idx):
    return [("ROWS", idx, 0), ("ROWS", idx, 1)]


def layernorm(k, z, zk, out_t, out_k, g_t, gk, b_t, bk, st6r, mvr, C):
    P = k.P
    st6, sk = st6r.next()
    mv, mk = mvr.next()
    P.op("dve", lambda e: e.bn_stats(out=st6[:, 0:6], in_=z[:, 0:512]), reads=[zk], writes=[sk + "a"])
    P.op("dve", lambda e: e.bn_stats(out=st6[:, 6:12], in_=z[:, 512:1024]), reads=[zk], writes=[sk + "b"])
    P.op("dve", lambda e: e.bn_aggr(out=mv[:, 0:2], in_=st6[:, 0:12]), reads=[sk + "a", sk + "b"], writes=[mk])
    P.op("act", lambda e: e.activation(out=mv[:, 2:3], in_=mv[:, 1:2], func=AF.Sqrt, bias=C["epsc"][:, 0:1], scale=1.0),
         reads=[mk, "epsc"], writes=[mk + "s"])
    P.op("dve", lambda e: e.reciprocal(out=mv[:, 3:4], in_=mv[:, 2:3]), reads=[mk + "s"], writes=[mk + "r"])
    P.op("dve", lambda e: e.tensor_scalar(out=out_t[:], in0=z[:], scalar1=mv[:, 0:1], scalar2=mv[:, 3:4],
                                          op0=ALU.subtract, op1=ALU.mult), reads=[zk, mk, mk + "r"], writes=[out_k])
    P.op("pool", lambda e: e.tensor_tensor(out=out_t[:], in0=out_t[:], in1=g_t[:], op=ALU.mult), reads=[out_k, gk], writes=[out_k])
    P.op("pool", lambda e: e.tensor_tensor(out=out_t[:], in0=out_t[:], in1=b_t[:], op=ALU.add), reads=[out_k, bk], writes=[out_k])


def modulate_T(k, xs, xk, scp, sck, sh, shk, hbr, hTr, C):
    P = k.P
    hb, hbk = hbr.next()
    hT, hTk = hTr.next()
    tpb, ident = C["tpb"], C["ident"]
    P.op("pool", lambda e: e.tensor_tensor(out=hb[0][:], in0=xs[:], in1=scp[:], op=ALU.mult), reads=[xk, sck], writes=[hbk + "f"])
    P.op("dve", lambda e: e.tensor_tensor(out=hb[1][:], in0=hb[0][:], in1=sh[:], op=ALU.add), reads=[hbk + "f", shk], writes=[hbk])

    def tr(e):
        for kc in range(8):
            ins = e.transpose(out=tpb[:, kc * 128:(kc + 1) * 128], in_=hb[1][:, kc * 128:(kc + 1) * 128], identity=ident[:])
        return ins
    P.op("pe", tr, reads=[hbk, "ident"], writes=["tpb"])
    P.op("act", lambda e: e.copy(out=hT[:].rearrange("p a b -> p (a b)"), in_=tpb[:]), reads=["tpb"], writes=[hTk])
    return hT, hTk


class PairRing:
    def __init__(self, k, name, n):
        self.t = [(k.sb(f"{name}f{i}", [128, D], F32), k.sb(f"{name}b{i}", [128, D], BF16)) for i in range(n)]
        self.keys = [f"{name}{i}" for i in range(n)]
        self.i = -1

    def next(self):
        self.i = (self.i + 1) % len(self.t)
        return self.t[self.i], self.keys[self.i]


def xrows(li, X, j):
    if li % 4 == 0 and j < 128:
        return X[0:NLAT, :].rearrange("(k2 k1) d -> k1 k2 d", k1=128)[j]
    return X[j * 128:(j + 1) * 128, :]


def layer(k, li, C):
    nc, P = k.nc, k.P
    kind = li % 4
    X, ROWS, H2T, GATES = C["X"], C["ROWS"], C["H2T"], C["GATES"]
    ada_rows(k, li, C)
    if checkpoint(1):
        return
    lnp = k.din(f"lnp{li}", [4, D])
    rw_in = k.din(f"router_w{li}", [D, NE])
    rb_in = k.din(f"router_b{li}", [1, NE])
    with_ctx = li < DEPTH - 1
    nsub_moe = NSUB if with_ctx else NSUB - 2

    with k.sub():
        if kind == 0:
            y_src = fourier_front(k, li, C)
        elif kind == 1:
            y_src = attn_front(k, li, C, dict(nkv=4, grp=4, p="fa", norm=True, sink=False, windowed=False))
        elif kind == 2:
            y_src = gmlp_front(k, li, C)
        else:
            y_src = attn_front(k, li, C, dict(nkv=2, grp=8, p="wa", norm=False, sink=True, windowed=True))
        if y_src is None:
            STOP[0] = 0

        with k.sub() if y_src is not None else contextlib.nullcontext():
            def row(name, src, rkeys=()):
                t = k.sb(name, [128, D])
                P.dma("sp", lambda e: e.dma_start(out=t[:], in_=src), reads=list(rkeys), writes=[name])
                return t
            g1 = [row(f"g1_{w}", ROWS[w * 6 + 2], rows_keys(w * 6 + 2)) for w in range(2)]
            sc2 = [row(f"sc2_{w}", ROWS[w * 6 + 4], rows_keys(w * 6 + 4)) for w in range(2)]
            sh2 = [row(f"sh2_{w}", ROWS[w * 6 + 3], rows_keys(w * 6 + 3)) for w in range(2)]
            lng = row("lng", lnp[0:1, :].to_broadcast([128, D]))
            lnb = row("lnb", lnp[1:2, :].to_broadcast([128, D]))
            rw = k.sb("rw", [128, 8, NE])
            with nc.allow_non_contiguous_dma(reason="small"):
                P.dma("sp", lambda e: e.dma_start(out=rw[:], in_=rw_in.rearrange("(kc p) e -> p kc e", p=128), allow_slow_non_contiguous=True), writes=["rw"])
            rb = k.sb("rb", [1, NE])
            P.dma("sp", lambda e: e.dma_start(out=rb[:], in_=rb_in), writes=["rb"])
            xr = Ring(k, "xs", 2, [128, D], F32)
            zr = Ring(k, "z", 2, [128, D], F32)
            x1r = Ring(k, "x1", 2, [128, D], F32)
            st6r = Ring(k, "st6", 2, [128, 12], F32)
            mvr = Ring(k, "mv", 2, [128, 4], F32)
            hbr = PairRing(k, "hb", 2)
            hTr = Ring(k, "hT", 2, [128, 8, 128], BF16)
            h32r = Ring(k, "h32", 2, [128, D], F32)
            hT32r = Ring(k, "hT32", 2, [128, 8, 128], F32)
            lgr = Ring(k, "lg", 2, [128, NE + 16], F32)
            for j in range(NSUB if y_src is not None else 0):
                w = 1 if j >= 128 else 0
                if w == 1 and not with_ctx:
                    continue
                xs, xk = xr.next()
                src = xrows(li, X, j)
                P.dma("sp", lambda e, xs=xs, src=src: e.dma_start(out=xs[:], in_=src), reads=["X", "Xc"], writes=[xk])
                z, zk = zr.next()
                y_halves = y_src(j)
                for hf in range(2):
                    pb, pk = y_halves[hf]
                    P.op("dve", lambda e, z=z, pb=pb, hf=hf, w=w: e.tensor_tensor(
                        out=z[:, hf * 512:(hf + 1) * 512], in0=pb[:], in1=g1[w][:, hf * 512:(hf + 1) * 512], op=ALU.mult),
                        reads=[pk, f"g1_{w}"], writes=[zk + str(hf)])
                P.op("dve", lambda e, z=z, xs=xs: e.scalar_tensor_tensor(
                    out=z[:], in0=xs[:], scalar=ALPHA, in1=z[:], op0=ALU.mult, op1=ALU.add),
                    reads=[xk, zk + "0", zk + "1"], writes=[zk])
                x1, x1k = x1r.next()
                layernorm(k, z, zk, x1, x1k, lng, "lng", lnb, "lnb", st6r, mvr, C)
                P.dma("sp", lambda e, x1=x1, src=src: e.dma_start(out=src, in_=x1[:]), reads=[x1k], writes=["X", "Xc"])
                hT, hTk = modulate_T(k, x1, x1k, sc2[w], f"sc2_{w}", sh2[w], f"sh2_{w}", hbr, hTr, C)
                P.dma("sp", lambda e, hT=hT, j=j: e.dma_start(out=H2T[j], in_=hT[:]), reads=[hTk], writes=[("H2T", j)])
                h32, h32k = h32r.next()
                P.op("pool", lambda e, h32=h32, x1=x1, w=w: e.tensor_tensor(out=h32[:], in0=x1[:], in1=sc2[w][:], op=ALU.mult),
                     reads=[x1k, f"sc2_{w}"], writes=[h32k + "a"])
                P.op("pool", lambda e, h32=h32, w=w: e.tensor_tensor(out=h32[:], in0=h32[:], in1=sh2[w][:], op=ALU.add),
                     reads=[h32k + "a", f"sh2_{w}"], writes=[h32k])
                hT32, hT32k = hT32r.next()
                for half in range(2):
                    pb, pk = k.bank()

                    def tr32(e, pb=pb, h32=h32, half=half):
                        for q in range(4):
                            kc = half * 4 + q
                            ins = e.matmul(pb[:, q * 128:(q + 1) * 128], lhsT=h32[:, kc * 128:(kc + 1) * 128], rhs=C["ident32"][:],
                                           start=True, stop=True)
                        return ins
                    P.op("pe", tr32, reads=[h32k, "ident32"], writes=[pk])
                    P.op("act", lambda e, pb=pb, hT32=hT32, half=half: e.copy(
                        out=hT32[:, half * 4:(half + 1) * 4, :].rearrange("p a b -> p (a b)"), in_=pb[:]), reads=[pk], writes=[hT32k + str(half)])
                pb, pk = k.bank()

                def rmm(e, pb=pb, hT32=hT32):
                    for kc in range(8):
                        e.matmul(pb[:, 0:NE], lhsT=hT32[:, kc, :], rhs=rw[:, kc, :], start=(kc == 0), stop=False)
                    return e.matmul(pb[:, 0:NE], lhsT=C["ones32"][0:1, :], rhs=rb[0:1, :], start=False, stop=True)
                P.op("pe", rmm, reads=[hT32k + "0", hT32k + "1", "rw", "rb", "ones32"], writes=[pk])
                lg, lgk = lgr.next()
                P.op("dve", lambda e, lg=lg, pb=pb: e.tensor_copy(out=lg[:, 0:NE], in_=pb[:, 0:NE]), reads=[pk], writes=[lgk + "l"])
                P.op("dve", lambda e, lg=lg: e.max(out=lg[:, NE:NE + 8], in_=lg[:, 0:NE]), reads=[lgk + "l"], writes=[lgk + "m"])
                P.op("dve", lambda e, lg=lg: e.tensor_scalar(out=lg[:, NE + 8:NE + 9], in0=lg[:, NE:NE + 1], scalar1=-1.0, scalar2=None,
                                                             op0=ALU.mult), reads=[lgk + "m"], writes=[lgk + "n"])
                gt, gtk = C["gring"].next()
                P.op("act", lambda e, lg=lg, gt=gt: e.activation(out=gt[:, 0:NE], in_=lg[:, 0:NE], func=AF.Exp,
                                                                bias=lg[:, NE + 8:NE + 9], scale=1.0), reads=[lgk + "l", lgk + "n"], writes=[gtk + "e"])
                P.op("dve", lambda e, lg=lg, gt=gt: e.scalar_tensor_tensor(
                    out=gt[:, 0:NE], in0=lg[:, 0:NE], scalar=lg[:, NE + 3:NE + 4], in1=gt[:, 0:NE], op0=ALU.is_ge, op1=ALU.mult),
                    reads=[lgk + "l", lgk + "m", gtk + "e"], writes=[gtk + "g"])
                P.op("dve", lambda e, gt=gt: e.tensor_reduce(out=gt[:, NE:NE + 1], in_=gt[:, 0:NE], axis=mybir.AxisListType.X, op=ALU.add),
                     reads=[gtk + "g"], writes=[gtk + "s"])
                P.op("dve", lambda e, gt=gt: e.reciprocal(out=gt[:, NE + 1:NE + 2], in_=gt[:, NE:NE + 1]), reads=[gtk + "s"], writes=[gtk + "r"])
                P.op("dve", lambda e, gt=gt: e.tensor_scalar(out=gt[:, 0:NE], in0=gt[:, 0:NE], scalar1=gt[:, NE + 1:NE + 2], scalar2=None,
                                                             op0=ALU.mult), reads=[gtk + "g", gtk + "r"], writes=[gtk])
                P.dma("sp", lambda e, gt=gt, j=j: e.dma_start(out=GATES[j], in_=gt[:, 0:NE]), reads=[gtk], writes=[("GATES", j)])

    if checkpoint(4):
        return
    moe(k, li, C, nsub_moe)


def fourier_front(k, li, C):
    nc, P = k.nc, k.P
    X, ROWS = C["X"], C["ROWS"]
    fw = k.din(f"fn_w{li}", [D, D])
    fb = k.din(f"fn_b{li}", [1, D])
    ccs_in = k.din("fn_ccs", [2, 256, 256])
    w128_in = k.din("fn_w128", [3, 128, 128])
    tw_in = k.din("fn_tw", [3, 128, 128])
    c8_in = k.din("fn_c8", [2, 256, 256])
    AB = k.dscr("fn_AB", [NTOK, 2 * D], BF16)
    VP = k.dscr("fn_VP", [128, 128, 2 * D], BF16)
    M12 = k.sb("M12", [128, 8, 2 * D], BF16)
    with k.sub():
        Wt = k.sb("fnW", [128, 8, D])
        P.dma("sp", lambda e: e.dma_start(out=Wt[:], in_=fw.rearrange("(kc p) n -> p kc n", p=128)), writes=["fnW"])
        ccs = k.sb("ccs", [128, 2, 2, 256])
        for t in range(2):
            P.dma("sp", lambda e, t=t: e.dma_start(out=ccs[:, t], in_=ccs_in[t].rearrange("(kk p) m -> p kk m", p=128)), writes=[("ccs", t)])
        for t in range(2):
            for rc in range(8):
                g = rc // 2
                for half in range(2):
                    pb, pk = k.bank()

                    def mm(e, pb=pb, t=t, rc=rc, g=g, half=half):
                        for kk in range(2):
                            ins = e.matmul(pb[:], lhsT=ccs[:, t, kk, (rc % 2) * 128:(rc % 2 + 1) * 128],
                                           rhs=Wt[:, g * 2 + kk, half * 512:(half + 1) * 512], start=(kk == 0), stop=(kk == 1))
                        return ins
                    P.op("pe", mm, reads=["fnW", ("ccs", t)], writes=[pk])
                    sc = (1.0 if t == 0 else -1.0) / 2048.0
                    P.op("dve", lambda e, pb=pb, t=t, rc=rc, half=half, sc=sc: e.tensor_scalar(
                        out=M12[:, rc, t * D + half * 512: t * D + (half + 1) * 512], in0=pb[:], scalar1=sc, scalar2=None, op0=ALU.mult),
                        reads=[pk], writes=[("M12", t, rc, half)])
    M12keys = [("M12", t, rc, half) for t in range(2) for rc in range(8) for half in range(2)]
    if checkpoint(1.5):
        return None

    with k.sub():
        def row(name, src, rkeys):
            t = k.sb(name, [128, D])
            P.dma("sp", lambda e: e.dma_start(out=t[:], in_=src), reads=list(rkeys), writes=[name])
            return t
        sc1 = [row(f"sc1_{w}", ROWS[w * 6 + 1], rows_keys(w * 6 + 1)) for w in range(2)]
        sh1 = [row(f"sh1_{w}", ROWS[w * 6 + 0], rows_keys(w * 6 + 0)) for w in range(2)]
        xr = Ring(k, "xsA", 3, [128, D], F32)
        hbr = PairRing(k, "hbA", 2)
        hTr = Ring(k, "hTA", 2, [128, 8, 128], BF16)
        abr = Ring(k, "abA", 2, [128, 2 * D], BF16)
        import os
        for j in range(int(os.environ.get("NSUB_A", NSUB))):
            w = 1 if j >= 128 else 0
            xs, xk = xr.next()
            P.dma("sp", lambda e, xs=xs, j=j: e.dma_start(out=xs[:], in_=X[j * 128:(j + 1) * 128, :]), reads=["X", "Xc"], writes=[xk])
            hT, hTk = modulate_T(k, xs, xk, sc1[w], f"sc1_{w}", sh1[w], f"sh1_{w}", hbr, hTr, C)
            ab, abk = abr.next()
            for ct in range(4):
                pb, pk = k.bank()

                def mm(e, pb=pb, hT=hT, ct=ct):
                    for kc in range(8):
                        ins = e.matmul(pb[:], lhsT=hT[:, kc, :], rhs=M12[:, kc, ct * 512:(ct + 1) * 512], start=(kc == 0), stop=(kc == 7))
                    return ins
                P.op("pe", mm, reads=[hTk] + M12keys, writes=[pk])
                if ct % 2 == 0:
                    P.op("act", lambda e, pb=pb, ab=ab, ct=ct: e.copy(out=ab[:, ct * 512:(ct + 1) * 512], in_=pb[:]), reads=[pk], writes=[abk + str(ct)])
                else:
                    P.op("dve", lambda e, pb=pb, ab=ab, ct=ct: e.tensor_copy(out=ab[:, ct * 512:(ct + 1) * 512], in_=pb[:]), reads=[pk], writes=[abk + str(ct)])
            P.dma("sp", lambda e, ab=ab, j=j: e.dma_start(out=AB[j * 128:(j + 1) * 128, :], in_=ab[:]),
                  reads=[abk + str(ct) for ct in range(4)], writes=[("AB", j)])
    ABkeys = [("AB", j) for j in range(128)]
    if checkpoint(2):
        return None

    w128 = k.sb("w128", [128, 3, 128], BF16)
    P.dma("pool", lambda e: e.dma_start(out=w128[:], in_=w128_in.rearrange("t p m -> p t m")), writes=["w128"])
    tw = k.sb("tw", [128, 3, 128])
    P.dma("sp", lambda e: e.dma_start(out=tw[:], in_=tw_in.rearrange("t p m -> p t m")), writes=["tw"])
    c8 = k.sb("c8", [128, 2, 2, 256], BF16)
    for t in range(2):
        P.dma("pool", lambda e, t=t: e.dma_start(out=c8[:, t], in_=c8_in[t].rearrange("(kk p) m -> p kk m", p=128)), writes=[("c8", t)])
    fbb = k.sb("fbb", [1, D], BF16)
    P.dma("pool", lambda e: e.dma_start(out=fbb[:], in_=fb), writes=["fbb"])

    with k.sub():
        ur = Ring(k, "u1", 2, [128, 2 * D], BF16)
        vr = Ring(k, "v1", 2, [128, 2 * D], BF16)
        tr_ = Ring(k, "t1", 8, [128, 512], F32)
        ABv = AB[0:NLAT, :].rearrange("(n1 n2) c -> n2 n1 c", n2=128)
        import os
        for n2 in range(int(os.environ.get("NSUB_1", 128))):
            u, uk = ur.next()
            P.dma("sp", lambda e, u=u, n2=n2: e.dma_start(out=u[:], in_=ABv[n2]), reads=ABkeys, writes=[uk])
            v, vk = vr.next()
            for half in range(2):
                sl = slice(half * 512, (half + 1) * 512)
                sli = slice(D + half * 512, D + (half + 1) * 512)
                pr, prk = k.bank()
                pi, pik = k.bank()

                def mmr(e, pr=pr, u=u, sl=sl, sli=sli):
                    e.matmul(pr[:], lhsT=w128[:, 0, :], rhs=u[:, sl], start=True, stop=False)
                    return e.matmul(pr[:], lhsT=w128[:, 1, :], rhs=u[:, sli], start=False, stop=True)

                def mmi(e, pi=pi, u=u, sl=sl, sli=sli):
                    e.matmul(pi[:], lhsT=w128[:, 0, :], rhs=u[:, sli], start=True, stop=False)
                    return e.matmul(pi[:], lhsT=w128[:, 2, :], rhs=u[:, sl], start=False, stop=True)
                P.op("pe", mmr, reads=[uk, "w128"], writes=[prk])
                P.op("pe", mmi, reads=[uk, "w128"], writes=[pik])
                t1, t1k = tr_.next()
                t2, t2k = tr_.next()
                t3, t3k = tr_.next()
                t4, t4k = tr_.next()
                for (tt, ttk, src, srck, col) in ((t1, t1k, pr, prk, 0), (t3, t3k, pi, pik, 1), (t2, t2k, pi, pik, 0), (t4, t4k, pr, prk, 2)):
                    P.op("dve", lambda e, tt=tt, src=src, col=col, n2=n2: e.tensor_scalar(
                        out=tt[:], in0=src[:], scalar1=tw[:, col, n2:n2 + 1], scalar2=None, op0=ALU.mult), reads=[srck, "tw"], writes=[ttk])
                P.op("pool", lambda e, v=v, t1=t1, t3=t3, sl=sl: e.tensor_tensor(out=v[:, sl], in0=t1[:], in1=t3[:], op=ALU.add),
                     reads=[t1k, t3k], writes=[vk + "r" + str(half)])
                P.op("pool", lambda e, v=v, t2=t2, t4=t4, sli=sli: e.tensor_tensor(out=v[:, sli], in0=t2[:], in1=t4[:], op=ALU.add),
                     reads=[t2k, t4k], writes=[vk + "i" + str(half)])
            P.dma("sp", lambda e, v=v, n2=n2: e.dma_start(out=VP[:, n2, :], in_=v[:]),
                  reads=[vk + a + str(h) for a in "ri" for h in range(2)], writes=[("VP", n2)])
    VPkeys = [("VP", n2) for n2 in range(128)]
    if checkpoint(3):
        return None

    state = {}

    def y_src(j):
        if "vpr" not in state:
            state["vpr"] = Ring(k, "vp2", 2, [128, 2 * D], BF16)
            state["abc"] = k.sb("abctx", [128, 2, 2 * D], BF16)
            P.dma("sp", lambda e: e.dma_start(out=state["abc"][:], in_=AB[NLAT:NTOK, :].rearrange("(kk p) c -> p kk c", p=128)),
                  reads=[("AB", 128), ("AB", 129)], writes=["abctx"])
        res = []
        if j < 128:
            vp, vpk = state["vpr"].next()
            P.dma("sp", lambda e: e.dma_start(out=vp[:], in_=VP[j]), reads=VPkeys, writes=[vpk])
            for half in range(2):
                pb, pk = k.bank()

                def mm(e, pb=pb, half=half):
                    e.matmul(pb[:], lhsT=w128[:, 0, :], rhs=vp[:, half * 512:(half + 1) * 512], start=True, stop=False)
                    e.matmul(pb[:], lhsT=w128[:, 1, :], rhs=vp[:, D + half * 512:D + (half + 1) * 512], start=False, stop=False)
                    return e.matmul(pb[:], lhsT=C["onesb"][0:1, :], rhs=fbb[0:1, half * 512:(half + 1) * 512], start=False, stop=True)
                P.op("pe", mm, reads=[vpk, "w128", "onesb", "fbb"], writes=[pk])
                res.append((pb, pk))
        else:
            kt = j - 128
            abc = state["abc"]
            for half in range(2):
                pb, pk = k.bank()

                def mm(e, pb=pb, half=half):
                    for kk in range(2):
                        e.matmul(pb[:], lhsT=c8[:, 0, kk, kt * 128:(kt + 1) * 128], rhs=abc[:, kk, half * 512:(half + 1) * 512],
                                 start=(kk == 0), stop=False)
                        e.matmul(pb[:], lhsT=c8[:, 1, kk, kt * 128:(kt + 1) * 128], rhs=abc[:, kk, D + half * 512:D + (half + 1) * 512],
                                 start=False, stop=False)
                    return e.matmul(pb[:], lhsT=C["onesb"][0:1, :], rhs=fbb[0:1, half * 512:(half + 1) * 512], start=False, stop=True)
                P.op("pe", mm, reads=["abctx", ("c8", 0), ("c8", 1), "onesb", "fbb"], writes=[pk])
                res.append((pb, pk))
        return res
    return y_src


def moe(k, li, C, nsub):
    nc, P = k.nc, k.P
    X, ROWS, H2T, GATES = C["X"], C["ROWS"], C["H2T"], C["GATES"]
    wgu_in = k.din(f"wgu{li}", [NE, D, 2 * D])
    bgu_in = k.din(f"bgu{li}", [NE, 2 * D])
    wd_in = k.din(f"wd{li}", [NE, D, D])
    bd_in = k.din(f"bd{li}", [NE, D])
    lnp = k.inputs[f"lnp{li}"]
    GS = 4
    with k.sub():
        def row(name, src, rkeys=()):
            t = k.sb(name, [128, D])
            P.dma("sp", lambda e: e.dma_start(out=t[:], in_=src), reads=list(rkeys), writes=[name])
            return t
        g2 = [row(f"g2_{w}", ROWS[w * 6 + 5], rows_keys(w * 6 + 5)) for w in range(2)]
        lng = row("lnfg", lnp[2:3, :].to_broadcast([128, D]))
        lnb = row("lnfb", lnp[3:4, :].to_broadcast([128, D]))
        bgu = k.sb("bgu", [128, NE, 8, 2])
        with nc.allow_non_contiguous_dma(reason="bias layout"):
            for e_ in range(NE):
                P.dma("sp", lambda e, e_=e_: e.dma_start(out=bgu[:, e_], in_=bgu_in[e_].rearrange("(c p j) -> p c j", p=128, j=2), allow_slow_non_contiguous=True),
                      writes=[("bgu", e_)])
        hTg = k.sb("hTg", [128, 8, GS, 128], BF16)
        Gg = k.sb("Gg", [128, GS, NE])
        acc = k.sb("acc", [128, GS, D])
        act = k.sb("actm", [128, 8, GS * 128], BF16)
        wgur = Ring(k, "wgu", 2, [128, 8, 2 * D], BF16)
        wdr = Ring(k, "wd", 2, [128, 8, D], BF16)
        bdr = Ring(k, "bd", 2, [1, D], BF16)
        tgr = Ring(k, "tg", 2, [128, 512], F32)
        tsr = Ring(k, "tsg", 2, [128, 512], F32)
        tlr = Ring(k, "tl", 2, [128, 512], F32)
        xr = Ring(k, "xsD", 2, [128, D], F32)
        zr = Ring(k, "zD", 2, [128, D], F32)
        x2r = Ring(k, "x2", 2, [128, D], F32)
        st6r = Ring(k, "st6D", 2, [128, 12], F32)
        mvr = Ring(k, "mvD", 2, [128, 4], F32)
        ngroups = (nsub + GS - 1) // GS
        for g in range(ngroups):
            j0 = g * GS
            ns = min(GS, nsub - j0)
            ntok = ns * 128
            for s_ in range(ns):
                P.dma("sp", lambda e, j0=j0, s_=s_: e.dma_start(out=hTg[:, :, s_, :], in_=H2T[j0 + s_]),
                      reads=[("H2T", j0 + s_)], writes=[("hTg", s_)])
            P.dma("sp", lambda e, j0=j0, ns=ns: e.dma_start(out=Gg[:, 0:ns, :], in_=GATES[j0:j0 + ns].rearrange("s p e -> p s e")),
                  reads=[("GATES", j) for j in range(j0, j0 + ns)], writes=["Gg"])
            P.op("pool", lambda e: e.memset(acc[:], 0.0), writes=[("acc", s, h) for s in range(GS) for h in range(2)])
            tchunks = [(t0, min(512, ntok - t0)) for t0 in range(0, ntok, 512)]
            for ex in range(NE):
                wgu, wguk = wgur.next()
                wd, wdk = wdr.next()
                bd, bdk = bdr.next()
                for hh in range(2):
                    P.dma("pool", lambda e, wgu=wgu, ex=ex, hh=hh: e.dma_start(
                        out=wgu[:, hh * 4:(hh + 1) * 4, :], in_=wgu_in[ex, hh * 512:(hh + 1) * 512, :].rearrange("(kc p) f -> p kc f", p=128)),
                        writes=[wguk + str(hh)])
                P.dma("pool", lambda e, wd=wd, ex=ex: e.dma_start(out=wd[:], in_=wd_in[ex].rearrange("(kc p) f -> p kc f", p=128)), writes=[wdk])
                P.dma("pool", lambda e, bd=bd, ex=ex: e.dma_start(out=bd[:], in_=bd_in[ex:ex + 1, :]), writes=[bdk])
                for c in range(8):
                    for (t0, tn) in tchunks:
                        pg, pgk = k.bank()
                        pl, plk = k.bank()
                        rhs_of = lambda kc, t0=t0, tn=tn: hTg[:, kc, t0 // 128:(t0 + tn) // 128, :].rearrange("p a b -> p (a b)")

                        def mmg(e, pg=pg, wgu=wgu, c=c, tn=tn, rhs_of=rhs_of):
                            for kc in range(8):
                                ins = e.matmul(pg[:, 0:tn], lhsT=wgu[:, kc, c * 256:(c + 1) * 256:2], rhs=rhs_of(kc), start=(kc == 0), stop=(kc == 7))
                            return ins

                        def mml(e, pl=pl, wgu=wgu, c=c, tn=tn, rhs_of=rhs_of):
                            for kc in range(8):
                                ins = e.matmul(pl[:, 0:tn], lhsT=wgu[:, kc, c * 256 + 1:(c + 1) * 256:2], rhs=rhs_of(kc), start=(kc == 0), stop=(kc == 7))
                            return ins
                        P.op("pe", mmg, reads=[wguk + "0", wguk + "1"] + [("hTg", s_) for s_ in range(ns)], writes=[pgk])
                        P.op("pe", mml, reads=[wguk + "0", wguk + "1"] + [("hTg", s_) for s_ in range(ns)], writes=[plk])
                        tg, tgk = tgr.next()
                        ts, tsk = tsr.next()
                        tl, tlk = tlr.next()
                        P.op("dve", lambda e, tg=tg, pg=pg, tn=tn, ex=ex, c=c: e.tensor_scalar(
                            out=tg[:, 0:tn], in0=pg[:, 0:tn], scalar1=bgu[:, ex, c, 0:1], scalar2=7.0, op0=ALU.add, op1=ALU.min),
                            reads=[pgk, ("bgu", ex)], writes=[tgk])
                        P.op("act", lambda e, ts=ts, tg=tg, tn=tn: e.activation(out=ts[:, 0:tn], in_=tg[:, 0:tn], func=AF.Sigmoid, scale=1.702),
                             reads=[tgk], writes=[tsk])
                        P.op("dve", lambda e, tl=tl, pl=pl, tn=tn, ex=ex, c=c: e.tensor_scalar(
                            out=tl[:, 0:tn], in0=pl[:, 0:tn], scalar1=bgu[:, ex, c, 1:2], scalar2=7.0, op0=ALU.add, op1=ALU.min),
                            reads=[plk, ("bgu", ex)], writes=[tlk])
                        P.op("pool", lambda e, tl=tl, tn=tn: e.tensor_scalar(
                            out=tl[:, 0:tn], in0=tl[:, 0:tn], scalar1=-7.0, scalar2=1.0, op0=ALU.max, op1=ALU.add), reads=[tlk], writes=[tlk])
                        P.op("pool", lambda e, tg=tg, ts=ts, tn=tn: e.tensor_tensor(out=tg[:, 0:tn], in0=tg[:, 0:tn], in1=ts[:, 0:tn], op=ALU.mult),
                             reads=[tgk, tsk], writes=[tgk])
                        P.op("dve", lambda e, tg=tg, tl=tl, c=c, t0=t0, tn=tn: e.tensor_tensor(
                            out=act[:, c, t0:t0 + tn], in0=tg[:, 0:tn], in1=tl[:, 0:tn], op=ALU.mult), reads=[tgk, tlk], writes=[("act", c, t0)])
                actkeys = [("act", c, t0) for c in range(8) for (t0, tn) in tchunks]
                for s in range(ns):
                    for half in range(2):
                        pb, pk = k.bank()

                        def mmd(e, pb=pb, wd=wd, bd=bd, s=s, half=half):
                            for fc in range(8):
                                e.matmul(pb[:], lhsT=act[:, fc, s * 128:(s + 1) * 128], rhs=wd[:, fc, half * 512:(half + 1) * 512], start=(fc == 0), stop=False)
                            return e.matmul(pb[:], lhsT=C["onesb"][0:1, :], rhs=bd[0:1, half * 512:(half + 1) * 512], start=False, stop=True)
                        P.op("pe", mmd, reads=actkeys + [wdk, bdk, "onesb"], writes=[pk])
                        tq, tqk = tgr.next()
                        P.op("dve", lambda e, pb=pb, s=s, ex=ex, tq=tq: e.tensor_scalar(
                            out=tq[:], in0=pb[:], scalar1=Gg[:, s, ex:ex + 1], scalar2=None, op0=ALU.mult), reads=[pk, "Gg"], writes=[tqk])
                        P.op("pool", lambda e, s=s, half=half, tq=tq: e.tensor_tensor(
                            out=acc[:, s, half * 512:(half + 1) * 512], in0=acc[:, s, half * 512:(half + 1) * 512], in1=tq[:], op=ALU.add),
                            reads=[tqk, ("acc", s, half)], writes=[("acc", s, half)])
            for s in range(ns):
                j = j0 + s
                w = 1 if j >= 128 else 0
                xs, xk = xr.next()
                src = xrows(li, X, j)
                P.dma("sp", lambda e, xs=xs, src=src: e.dma_start(out=xs[:], in_=src), reads=["X", "Xc"], writes=[xk])
                z, zk = zr.next()
                P.op("dve", lambda e, z=z, s=s, w=w: e.tensor_tensor(out=z[:], in0=acc[:, s, :], in1=g2[w][:], op=ALU.mult),
                     reads=[("acc", s, 0), ("acc", s, 1), f"g2_{w}"], writes=[zk + "a"])
                P.op("dve", lambda e, z=z, xs=xs: e.scalar_tensor_tensor(out=z[:], in0=xs[:], scalar=ALPHA, in1=z[:], op0=ALU.mult, op1=ALU.add),
                     reads=[xk, zk + "a"], writes=[zk])
                x2, x2k = x2r.next()
                layernorm(k, z, zk, x2, x2k, lng, "lnfg", lnb, "lnfb", st6r, mvr, C)
                P.dma("sp", lambda e, x2=x2, src=src: e.dma_start(out=src, in_=x2[:]), reads=[x2k], writes=["X", "Xc"])


def _tables():
    t = {}
    i256 = np.arange(256)
    ang = 2 * np.pi * np.outer(i256, i256) / 256.0
    t["fn_ccs"] = np.stack([np.cos(ang), np.sin(ang)]).astype(np.float32)
    t["fn_c8"] = (8.0 * np.stack([np.cos(ang), np.sin(ang)])).astype(np.float32)
    i128 = np.arange(128)
    a128 = 2 * np.pi * np.outer(i128, i128) / 128.0
    t["fn_w128"] = np.stack([np.cos(a128), np.sin(a128), -np.sin(a128)]).astype(np.float32)
    atw = 2 * np.pi * np.outer(i128, i128) / 16384.0
    t["fn_tw"] = np.stack([np.cos(atw), np.sin(atw), -np.sin(atw)]).astype(np.float32)
    t["ident"] = np.eye(128, dtype=np.float32)
    rows = NLAT // 64
    row = np.repeat(np.arange(rows, dtype=np.float32), 64)
    col = np.tile(np.arange(64, dtype=np.float32), rows)
    inv = (np.float32(10000.0) ** (-np.arange(16, dtype=np.float32) / np.float32(16))).astype(np.float32)
    ang = np.concatenate([row[:, None] * inv, col[:, None] * inv], axis=-1).astype(np.float32)
    t["rope"] = np.stack([np.tile(np.cos(ang), (1, 16)), np.tile(np.sin(ang), (1, 16))]).astype(np.float32)
    kk = np.arange(128)[:, None]
    qq = np.arange(128)[None, :]
    t["wa_mask"] = np.stack([np.tile((kk >= qq), (1, 4)), np.tile((kk <= qq), (1, 4))]).astype(np.float32)
    return t


_CACHE = {}


def kernel(**inp):
    layers = list(range(DEPTH))
    if "k" not in _CACHE:
        _CACHE["k"] = build(layers)
    k = _CACHE["k"]
    f = lambda a: np.ascontiguousarray(np.asarray(a, dtype=np.float32))
    m = dict(_tables())
    m["x"] = f(inp["x"]).reshape(NLAT, D)
    m["ctx"] = f(inp["ctx"]).reshape(NCTX, D)
    m["cvec"] = np.stack([f(inp["c"]).reshape(D), f(inp["c_ctx"]).reshape(D)])
    for li in layers:
        m[f"ada_w{li}"] = f(inp["ada_w"][li])
        m[f"ada_b{li}"] = f(inp["ada_b"][li]).reshape(1, -1)
        m[f"lnp{li}"] = np.stack([f(inp["ln_mix_g"][li]), f(inp["ln_mix_b"][li]), f(inp["ln_ffn_g"][li]), f(inp["ln_ffn_b"][li])])
        m[f"router_w{li}"] = f(inp["router_w"][li])
        m[f"router_b{li}"] = f(inp["router_b"][li]).reshape(1, -1)
        m[f"wgu{li}"] = f(inp["exp_w_gate_up"][li])
        m[f"bgu{li}"] = f(inp["exp_b_gate_up"][li])
        m[f"wd{li}"] = f(inp["exp_w_down"][li])
        m[f"bd{li}"] = f(inp["exp_b_down"][li])
        jj = li // 4
        if li % 4 == 0:
            m[f"fn_w{li}"] = f(inp["fn_w_out"][jj])
            m[f"fn_b{li}"] = f(inp["fn_b_out"][jj]).reshape(1, -1)
        elif li % 4 == 1:
            m[f"fa_wqkv{li}"] = f(inp["fa_w_qkv"][jj])
            m[f"fa_bqkv{li}"] = f(inp["fa_b_qkv"][jj]).reshape(1, -1)
            m[f"fa_wo{li}"] = f(inp["fa_w_out"][jj])
            m[f"fa_bo{li}"] = f(inp["fa_b_out"][jj]).reshape(1, -1)
            m[f"fa_qn{li}"] = np.tile(f(inp["fa_q_norm"][jj]), 16).reshape(1, -1)
            m[f"fa_kn{li}"] = np.tile(f(inp["fa_k_norm"][jj]), 4).reshape(1, -1)
        elif li % 4 == 2:
            m[f"gm_win{li}"] = f(inp["gm_w_in"][jj])
            m[f"gm_bin{li}"] = f(inp["gm_b_in"][jj]).reshape(1, -1)
            m[f"gm_vng{li}"] = f(inp["gm_v_norm_g"][jj]).reshape(1, -1)
            m[f"gm_vnb{li}"] = f(inp["gm_v_norm_b"][jj]).reshape(1, -1)
            m[f"gm_wsT{li}"] = f(np.transpose(np.asarray(inp["gm_w_s"][jj]), (0, 2, 1)))
            m[f"gm_bs{li}"] = f(inp["gm_b_s"][jj]).reshape(1, -1)
            m[f"gm_wo{li}"] = f(inp["gm_w_out"][jj])
            m[f"gm_bo{li}"] = f(inp["gm_b_out"][jj]).reshape(1, -1)
        else:
            m[f"wa_wqkv{li}"] = f(inp["wa_w_qkv"][jj])
            m[f"wa_bqkv{li}"] = f(inp["wa_b_qkv"][jj]).reshape(1, -1)
            m[f"wa_wo{li}"] = f(inp["wa_w_out"][jj])
            m[f"wa_bo{li}"] = f(inp["wa_b_out"][jj]).reshape(1, -1)
            m[f"wa_sink{li}"] = np.repeat(f(inp["wa_sink"][jj]), 128).reshape(1, -1)
    m = {n: m[n] for n in k.inputs}
    res = run_bass_kernel_spmd(k.nc, [m], core_ids=[0])
    return np.asarray(res.results[0]["out"], dtype=np.float32).reshape(1, NLAT, D)


def stage_a(k, li, C, body):
    P = k.P
    X, ROWS = C["X"], C["ROWS"]
    sc1 = k.sb("sc1", [128, D])
    sh1 = k.sb("sh1", [128, D])

    def load_rows(w):
        P.dma("sp", lambda e: e.dma_start(out=sc1[:], in_=ROWS[w * 6 + 1]), reads=rows_keys(w * 6 + 1), writes=["sc1"])
        P.dma("sp", lambda e: e.dma_start(out=sh1[:], in_=ROWS[w * 6 + 0]), reads=rows_keys(w * 6 + 0), writes=["sh1"])
    xr = Ring(k, "xsA", 3, [128, D], F32)
    hbr = PairRing(k, "hbA", 2)
    hTr = Ring(k, "hTA", 2, [128, 8, 128], BF16)
    import os
    for j in (list(range(NSUB)) if "NSUB_A" not in os.environ else [0, 129][:int(os.environ["NSUB_A"])]):
        if j == 0:
            load_rows(0)
        if j == 128:
            load_rows(1)
        xs, xk = xr.next()
        P.dma("sp", lambda e, xs=xs, j=j: e.dma_start(out=xs[:], in_=X[j * 128:(j + 1) * 128, :]), reads=["X", "Xc"], writes=[xk])
        hT, hTk = modulate_T(k, xs, xk, sc1, "sc1", sh1, "sh1", hbr, hTr, C)
        body(j, hT, hTk)


def attn_front(k, li, C, cfg):
    nc, P = k.nc, k.P
    nkv, grp, p = cfg["nkv"], cfg["grp"], cfg["p"]
    norm, sink, windowed = cfg["norm"], cfg["sink"], cfg["windowed"]
    with_ctx = li < DEPTH - 1
    KW = nkv * 64
    QKVW = D + 2 * KW
    wq_in = k.din(f"{p}_wqkv{li}", [D, QKVW])
    bq_in = k.din(f"{p}_bqkv{li}", [1, QKVW])
    wo_in = k.din(f"{p}_wo{li}", [D, D])
    bo_in = k.din(f"{p}_bo{li}", [1, D])
    if "rope_in" not in C:
        C["rope_in"] = k.din("rope", [2, NLAT, 512])
    rope_in = C["rope_in"]
    if norm:
        qn_in = k.din(f"{p}_qn{li}", [1, D])
        kn_in = k.din(f"{p}_kn{li}", [1, KW])
    if sink:
        sink_in = k.din(f"{p}_sink{li}", [1, 2048])
    if windowed:
        mask_in = k.din(f"{p}_mask", [2, 128, 512])
    Q = k.dscr(f"{p}_Q{li}", [NTOK, D], BF16)
    KT = k.dscr(f"{p}_KT{li}", [nkv, 64, NTOK], BF16)
    V = k.dscr(f"{p}_V{li}", [NTOK, KW], BF16)
    OT = k.dscr(f"{p}_OT{li}", [NSUB, 64, 16, 128], BF16)
    tpb, ident = C["tpb"], C["ident"]

    with k.sub():
        wq = k.sb("wq", [128, 8, QKVW], BF16)
        for hh in range(2):
            P.dma("pool", lambda e, hh=hh: e.dma_start(out=wq[:, hh * 4:(hh + 1) * 4, :],
                                                      in_=wq_in[hh * 512:(hh + 1) * 512, :].rearrange("(kc p) f -> p kc f", p=128)),
                  writes=[("wq", hh)])
        bq = k.sb("bq", [1, QKVW], BF16)
        P.dma("pool", lambda e: e.dma_start(out=bq[:], in_=bq_in), writes=["bq"])
        if norm:
            gq = k.sb("gq", [128, D])
            P.dma("sp", lambda e: e.dma_start(out=gq[:], in_=qn_in.to_broadcast([128, D])), writes=["gq"])
            gk = k.sb("gk", [128, KW])
            P.dma("sp", lambda e: e.dma_start(out=gk[:], in_=kn_in.to_broadcast([128, KW])), writes=["gk"])
        NH = 16 + nkv
        qkr = Ring(k, "qkraw", 2, [128, D + KW], F32)
        qkn = Ring(k, "qkn", 2, [128, D + KW], F32)
        sqr = Ring(k, "sq", 1, [128, D + KW], F32)
        ssr = Ring(k, "ss", 2, [128, 2 * NH], F32)
        vtr = Ring(k, "vt", 2, [128, KW], BF16)
        qrr = Ring(k, "qr", 2, [128, D + KW], BF16)
        csr = Ring(k, "cs", 2, [128, 2, 512], F32)
        tmr = Ring(k, "ropet", 4, [128, (D + KW) // 2], F32)
        ktr = Ring(k, "kTt", 2, [64, nkv * 128], BF16)
        HP = (D + KW) // 2
        colsets = [(0, 512), (512, 1024), (1024, QKVW)]

        def body(j, hT, hTk):
            import os
            if "p" in os.environ.get("ASKIP", ""):
                return
            raw, rawk = qkr.next()
            vt, vtk = vtr.next()
            for ci, (c0, c1) in enumerate(colsets):
                pb, pk = k.bank()
                wdt = c1 - c0

                def mm(e, pb=pb, c0=c0, c1=c1, wdt=wdt):
                    for kc in range(8):
                        e.matmul(pb[:, 0:wdt], lhsT=hT[:, kc, :], rhs=wq[:, kc, c0:c1], start=(kc == 0), stop=False)
                    return e.matmul(pb[:, 0:wdt], lhsT=C["onesb"][0:1, :], rhs=bq[0:1, c0:c1], start=False, stop=True)
                P.op("pe", mm, reads=[hTk, ("wq", 0), ("wq", 1), "bq", "onesb"], writes=[pk])
                if "E" in os.environ.get("ASKIP", ""):
                    continue
                if ci == 0:
                    P.op("act", lambda e, pb=pb, raw=raw: e.copy(out=raw[:, 0:512], in_=pb[:]), reads=[pk], writes=[rawk + "0"])
                elif ci == 1:
                    P.op("dve", lambda e, pb=pb, raw=raw: e.tensor_copy(out=raw[:, 512:1024], in_=pb[:]), reads=[pk], writes=[rawk + "1"])
                else:
                    P.op("dve", lambda e, pb=pb, raw=raw: e.tensor_copy(out=raw[:, D:D + KW], in_=pb[:, 0:KW]), reads=[pk], writes=[rawk + "2"])
                    P.op("dve", lambda e, pb=pb, vt=vt: e.tensor_copy(out=vt[:], in_=pb[:, KW:2 * KW]), reads=[pk], writes=[vtk])
                    if "V" not in os.environ.get("ASKIP", ""):
                        P.dma("sp", lambda e, vt=vt, j=j: e.dma_start(out=V[j * 128:(j + 1) * 128, :], in_=vt[:]), reads=[vtk], writes=[("V", j)])
            if "c" in os.environ.get("ASKIP", ""):
                return
            rawkeys = [rawk + "0", rawk + "1", rawk + "2"]
            import os
            SK = os.environ.get("ASKIP", "")
            if norm and "n" not in SK:
                sq, sqk = sqr.next()
                ss, ssk = ssr.next()
                qn_, qnk = qkn.next()
                P.op("act", lambda e, sq=sq, raw=raw: e.activation(out=sq[:], in_=raw[:], func=AF.Square), reads=rawkeys, writes=[sqk])
                P.op("dve", lambda e, sq=sq, ss=ss: e.tensor_reduce(out=ss[:, 0:NH], in_=sq[:].rearrange("p (h d) -> p h d", d=64),
                                                                  axis=mybir.AxisListType.X, op=ALU.add), reads=[sqk], writes=[ssk + "a"])
                P.op("dve", lambda e, ss=ss: e.tensor_scalar(out=ss[:, 0:NH], in0=ss[:, 0:NH], scalar1=1.0 / 64.0, scalar2=1e-6,
                                                             op0=ALU.mult, op1=ALU.add), reads=[ssk + "a"], writes=[ssk + "b"])
                P.op("act", lambda e, ss=ss: e.activation(out=ss[:, 0:NH], in_=ss[:, 0:NH], func=AF.Sqrt), reads=[ssk + "b"], writes=[ssk + "c"])
                P.op("dve", lambda e, ss=ss: e.reciprocal(out=ss[:, NH:2 * NH], in_=ss[:, 0:NH]), reads=[ssk + "c"], writes=[ssk])
                P.op("pool", lambda e, raw=raw: e.tensor_tensor(out=raw[:, 0:D], in0=raw[:, 0:D], in1=gq[:], op=ALU.mult),
                     reads=rawkeys + ["gq", sqk], writes=[rawk + "g"])
                P.op("pool", lambda e, raw=raw: e.tensor_tensor(out=raw[:, D:D + KW], in0=raw[:, D:D + KW], in1=gk[:], op=ALU.mult),
                     reads=rawkeys + ["gk", sqk], writes=[rawk + "h"])
                hkeys = []
                for h in range(NH):
                    eng = "dve" if h % 2 == 0 else "pool"
                    P.op(eng, lambda e, h=h, raw=raw, qn_=qn_, ss=ss: e.tensor_scalar(
                        out=qn_[:, h * 64:(h + 1) * 64], in0=raw[:, h * 64:(h + 1) * 64], scalar1=ss[:, NH + h:NH + h + 1], scalar2=None,
                        op0=ALU.mult), reads=[rawk + "g", rawk + "h", ssk], writes=[(qnk, h)])
                    hkeys.append((qnk, h))
                src, srckeys = qn_, hkeys
            else:
                src, srckeys = raw, rawkeys
            qr, qrk = qrr.next()
            if j < 128 and "r" not in SK:
                cs, csk = csr.next()
                P.dma("sp", lambda e, cs=cs, j=j: e.dma_start(out=cs[:], in_=rope_in[:, j * 128:(j + 1) * 128, :].rearrange("t p f -> p t f")),
                      writes=[csk])
                sv = src[:].rearrange("p (i two) -> p i two", two=2)
                ov = qr[:].rearrange("p (i two) -> p i two", two=2)
                segs = [(0, 512, 0), (512, HP, 0)]
                ta, tak = tmr.next()
                tb, tbk = tmr.next()
                tc_, tck = tmr.next()
                td, tdk = tmr.next()
                for (p0, p1, t0) in segs:
                    n = p1 - p0
                    sfx = str(p0)
                    P.op("dve", lambda e, p0=p0, p1=p1, t0=t0, n=n: e.tensor_tensor(out=ta[:, p0:p1], in0=sv[:, p0:p1, 0], in1=cs[:, 0, t0:t0 + n], op=ALU.mult),
                         reads=srckeys + [csk], writes=[tak + sfx])
                    P.op("pool", lambda e, p0=p0, p1=p1, t0=t0, n=n: e.tensor_tensor(out=tb[:, p0:p1], in0=sv[:, p0:p1, 1], in1=cs[:, 1, t0:t0 + n], op=ALU.mult),
                         reads=srckeys + [csk], writes=[tbk + sfx])
                    P.op("pool", lambda e, p0=p0, p1=p1, t0=t0, n=n: e.tensor_tensor(out=tc_[:, p0:p1], in0=sv[:, p0:p1, 0], in1=cs[:, 1, t0:t0 + n], op=ALU.mult),
                         reads=srckeys + [csk], writes=[tck + sfx])
                    P.op("dve", lambda e, p0=p0, p1=p1, t0=t0, n=n: e.tensor_tensor(out=td[:, p0:p1], in0=sv[:, p0:p1, 1], in1=cs[:, 0, t0:t0 + n], op=ALU.mult),
                         reads=srckeys + [csk], writes=[tdk + sfx])
                    P.op("dve", lambda e, p0=p0, p1=p1: e.tensor_tensor(out=ov[:, p0:p1, 0], in0=ta[:, p0:p1], in1=tb[:, p0:p1], op=ALU.subtract),
                         reads=[tak + sfx, tbk + sfx], writes=[qrk + "a" + sfx])
                    P.op("pool", lambda e, p0=p0, p1=p1: e.tensor_tensor(out=ov[:, p0:p1, 1], in0=tc_[:, p0:p1], in1=td[:, p0:p1], op=ALU.add),
                         reads=[tck + sfx, tdk + sfx], writes=[qrk + "b" + sfx])
                qrkeys = [qrk + a + str(p0) for a in "ab" for (p0, _, _) in segs]
            else:
                P.op("act", lambda e: e.copy(out=qr[:], in_=src[:]), reads=srckeys, writes=[qrk])
                qrkeys = [qrk]
            if "q" not in SK:
                P.dma("sp", lambda e, j=j: e.dma_start(out=Q[j * 128:(j + 1) * 128, :], in_=qr[:, 0:D]), reads=qrkeys, writes=[("Q", j)])
            if "k" in SK:
                return
            kTt, kTk = ktr.next()

            def trk(e):
                for hk in range(nkv):
                    ins = e.transpose(out=tpb[0:64, hk * 128:(hk + 1) * 128], in_=qr[:, D + hk * 64:D + (hk + 1) * 64], identity=ident[:])
                return ins
            P.op("pe", trk, reads=qrkeys + ["ident"], writes=["tpb"])
            P.op("act", lambda e: e.copy(out=kTt[:], in_=tpb[0:64, 0:nkv * 128]), reads=["tpb"], writes=[kTk])
            P.dma("sp", lambda e, j=j: e.dma_start(out=KT[:, :, j * 128:(j + 1) * 128].rearrange("h d t -> d h t"),
                                                 in_=kTt[:].rearrange("d (h t) -> d h t", h=nkv)), reads=[kTk], writes=[("KT", j)])
        stage_a(k, li, C, body)
    KTkeys = [("KT", j) for j in range(NSUB)]
    Vkeys = [("V", j) for j in range(NSUB)]
    if checkpoint(5):
        return None

    with k.sub():
        kTs = k.sb("kTs", [64, NTOK], BF16)
        vext = k.sb("vext", [128, NSUB, 65], BF16)
        P.op("pool", lambda e: e.memset(vext[:, :, 64:65], 1.0), writes=["vones"])
        if windowed:
            masks = k.sb("wmask", [128, 2, 512], BF16)
            P.dma("pool", lambda e: e.dma_start(out=masks[:], in_=mask_in.rearrange("t p f -> p t f")), writes=["wmask"])
        if sink:
            es = k.sb("es", [65, 2048])
            P.dma("sp", lambda e: e.dma_start(out=es[64:65, :], in_=sink_in), writes=["es0"])
            P.op("act", lambda e: e.activation(out=es[64:65, :], in_=es[64:65, :], func=AF.Exp), reads=["es0"], writes=["es"])
        GW = grp * 64
        qtr = Ring(k, "qt", 2, [128, GW], BF16)
        qTr = Ring(k, "qT", 2, [64, 512], BF16)
        ptr_ = Ring(k, "pt", 3, [128, 512], BF16)
        rdr = Ring(k, "rd", 2, [65, 512], F32)
        bcr = Ring(k, "bcs", 2, [64, 512], F32)
        otr = Ring(k, "ot", 2, [64, 512], BF16)
        qtiles = list(range(128)) + ([128, 129] if with_ctx else [])
        import os
        if "QT" in os.environ:
            qtiles = qtiles[:int(os.environ["QT"])]
        Vv = V.rearrange("(t p) c -> p t c", p=128)
        for hk in range(nkv):
            for q4 in range(5):
                t0, t1 = q4 * 26 * 128, (q4 + 1) * 26 * 128
                P.dma("sp", lambda e, hk=hk, t0=t0, t1=t1: e.dma_start(out=kTs[:, t0:t1], in_=KT[hk, :, t0:t1]), reads=KTkeys, writes=[("kTs", q4)])
                P.dma("sp", lambda e, hk=hk, q4=q4: e.dma_start(out=vext[:, q4 * 26:(q4 + 1) * 26, 0:64], in_=Vv[:, q4 * 26:(q4 + 1) * 26, hk * 64:(hk + 1) * 64]),
                      reads=Vkeys, writes=[("vext", q4)])
            kvkeys = [("kTs", q4) for q4 in range(5)] + [("vext", q4) for q4 in range(5)] + ["vones"]
            for i in qtiles:
                qt, qtk = qtr.next()
                P.dma("sp", lambda e, qt=qt, i=i, hk=hk: e.dma_start(out=qt[:], in_=Q[i * 128:(i + 1) * 128, hk * GW:(hk + 1) * GW]),
                      reads=[("Q", i)], writes=[qtk])
                if i >= 128:
                    kts = [(128, None), (129, None)]
                elif windowed:
                    kts = ([(i - 1, 0)] if i > 0 else []) + [(i, None)] + ([(i + 1, 1)] if i < 127 else []) + [(128, None), (129, None)]
                else:
                    kts = [(t, None) for t in range(NSUB)]
                for gh in range(grp // 4):
                    h0 = hk * grp + gh * 4
                    qT, qTk = qTr.next()

                    def trq(e, qt=qt, gh=gh):
                        for g in range(4):
                            ins = e.transpose(out=tpb[0:64, g * 128:(g + 1) * 128], in_=qt[:, (gh * 4 + g) * 64:(gh * 4 + g + 1) * 64], identity=ident[:])
                        return ins
                    P.op("pe", trq, reads=[qtk, "ident"], writes=["tpb"])
                    P.op("act", lambda e, qT=qT: e.copy(out=qT[:], in_=tpb[0:64, 0:512]), reads=["tpb"], writes=[qTk])
                    acc, acck = k.accs.next()
                    for idx, (kt, mk_) in enumerate(kts):
                        sbk, sk_ = k.bank()
                        P.op("pe", lambda e, sbk=sbk, kt=kt, qT=qT: e.matmul(sbk[:], lhsT=kTs[:, kt * 128:(kt + 1) * 128], rhs=qT[:], start=True, stop=True),
                             reads=kvkeys[0:5] + [qTk], writes=[sk_])
                        pt, ptk = ptr_.next()
                        P.op("act", lambda e, pt=pt, sbk=sbk: e.activation(out=pt[:], in_=sbk[:], func=AF.Exp, scale=0.125), reads=[sk_], writes=[ptk])
                        if mk_ is not None:
                            P.op("pool", lambda e, pt=pt, mk_=mk_: e.tensor_tensor(out=pt[:], in0=pt[:], in1=masks[:, mk_, :], op=ALU.mult),
                                 reads=[ptk, "wmask"], writes=[ptk])
                        P.op("pe", lambda e, acc=acc, kt=kt, pt=pt, idx=idx, n=len(kts): e.matmul(
                            acc[0:65, :], lhsT=vext[:, kt, :], rhs=pt[:], start=(idx == 0), stop=(idx == n - 1)),
                            reads=kvkeys[5:] + [ptk], writes=[acck])
                    rd, rdk = rdr.next()
                    if sink:
                        P.op("dve", lambda e, rd=rd, acc=acc, h0=h0: e.tensor_tensor(out=rd[64:65, :], in0=acc[64:65, :], in1=es[64:65, h0 * 128:(h0 + 4) * 128], op=ALU.add),
                             reads=[acck, "es"], writes=[rdk + "s"])
                        P.op("dve", lambda e, rd=rd: e.reciprocal(out=rd[64:65, :], in_=rd[64:65, :]), reads=[rdk + "s"], writes=[rdk])
                    else:
                        P.op("dve", lambda e, rd=rd, acc=acc: e.reciprocal(out=rd[64:65, :], in_=acc[64:65, :]), reads=[acck], writes=[rdk])
                    bb, bbk = k.bank()
                    P.op("pe", lambda e, bb=bb, rd=rd: e.matmul(bb[0:64, :], lhsT=C["ones32"][64:65, 0:64], rhs=rd[64:65, :], start=True, stop=True),
                         reads=[rdk, "ones32"], writes=[bbk])
                    bcs, bck = bcr.next()
                    P.op("act", lambda e, bcs=bcs, bb=bb: e.copy(out=bcs[:], in_=bb[0:64, :]), reads=[bbk], writes=[bck])
                    ot, otk = otr.next()
                    P.op("dve", lambda e, ot=ot, acc=acc, bcs=bcs: e.tensor_tensor(out=ot[:], in0=acc[0:64, :], in1=bcs[:], op=ALU.mult),
                         reads=[acck, bck], writes=[otk])
                    P.dma("sp", lambda e, ot=ot, i=i, h0=h0: e.dma_start(out=OT[i][:, h0:h0 + 4, :], in_=ot[:].rearrange("d (g t) -> d g t", g=4)),
                          reads=[otk], writes=[("OT", i, h0)])

    if checkpoint(6):
        return None
    wo = k.sb("wo", [64, 16, D], BF16)
    for hh in range(2):
        P.dma("pool", lambda e, hh=hh: e.dma_start(out=wo[:, hh * 8:(hh + 1) * 8, :],
                                                  in_=wo_in[hh * 512:(hh + 1) * 512, :].rearrange("(h p) n -> p h n", p=64)), writes=[("wo", hh)])
    bo = k.sb("bo", [1, D], BF16)
    P.dma("pool", lambda e: e.dma_start(out=bo[:], in_=bo_in), writes=["bo"])
    state = {}

    def y_src(j):
        if "r" not in state:
            state["r"] = Ring(k, "oTt", 2, [64, 16, 128], BF16)
        oT, oTk = state["r"].next()
        P.dma("sp", lambda e: e.dma_start(out=oT[:], in_=OT[j]), reads=[("OT", j, h0) for h0 in range(0, 16, 4)], writes=[oTk])
        res = []
        for half in range(2):
            pb, pk = k.bank()

            def mm(e, pb=pb, half=half):
                for h in range(16):
                    e.matmul(pb[:], lhsT=oT[:, h, :], rhs=wo[:, h, half * 512:(half + 1) * 512], start=(h == 0), stop=False)
                return e.matmul(pb[:], lhsT=C["onesb"][0:1, :], rhs=bo[0:1, half * 512:(half + 1) * 512], start=False, stop=True)
            P.op("pe", mm, reads=[oTk, ("wo", 0), ("wo", 1), "bo", "onesb"], writes=[pk])
            res.append((pb, pk))
        return res
    return y_src


def gmlp_front(k, li, C):
    nc, P = k.nc, k.P
    win_in = k.din(f"gm_win{li}", [D, 4096])
    bin_in = k.din(f"gm_bin{li}", [1, 4096])
    vng_in = k.din(f"gm_vng{li}", [1, 2048])
    vnb_in = k.din(f"gm_vnb{li}", [1, 2048])
    wsT_in = k.din(f"gm_wsT{li}", [8, 128, 128])
    bs_in = k.din(f"gm_bs{li}", [1, 1024])
    wo_in = k.din(f"gm_wo{li}", [2048, D])
    bo_in = k.din(f"gm_bo{li}", [1, D])
    UVT = k.dscr(f"gm_UVT{li}", [NSUB, 128, 16, 128], BF16)
    with k.sub():
        win = k.sb("win", [128, 8, 4096], BF16)
        for kc in range(8):
            P.dma("pool", lambda e, kc=kc: e.dma_start(out=win[:, kc, :], in_=win_in[kc * 128:(kc + 1) * 128, :]), writes=[("win", kc)])
        winkeys = [("win", kc) for kc in range(8)]
        binT = k.sb("binT", [128, 16])
        P.dma("sp", lambda e: e.dma_start(out=binT[:], in_=bin_in[0, 0:2048].rearrange("(ct p) -> p ct", p=128), allow_slow_non_contiguous=True),
              writes=["binT"])
        bvb = k.sb("bvb", [1, 2048], BF16)
        P.dma("pool", lambda e: e.dma_start(out=bvb[:], in_=bin_in[:, 2048:4096]), writes=["bvb"])
        vng = k.sb("vng", [128, 2048])
        P.dma("sp", lambda e: e.dma_start(out=vng[:], in_=vng_in.to_broadcast([128, 2048])), writes=["vng"])
        vnb = k.sb("vnb", [128, 2048])
        P.dma("sp", lambda e: e.dma_start(out=vnb[:], in_=vnb_in.to_broadcast([128, 2048])), writes=["vnb"])
        wsT = k.sb("wsT", [128, 8, 128], BF16)
        P.dma("pool", lambda e: e.dma_start(out=wsT[:], in_=wsT_in.rearrange("h q p -> q h p")), writes=["wsT"])
        bs32 = k.sb("bs32", [1, 1024])
        P.dma("sp", lambda e: e.dma_start(out=bs32[:], in_=bs_in), writes=["bs32"])
        bsh = k.sb("bsh", [1, 1024], BF16)
        bsl = k.sb("bsl", [1, 1024], BF16)
        bst = k.sb("bst", [1, 1024])
        P.op("act", lambda e: e.copy(out=bsh[:], in_=bs32[:]), reads=["bs32"], writes=["bsh"])
        P.op("dve", lambda e: e.tensor_copy(out=bst[:], in_=bsh[:]), reads=["bsh"], writes=["bst"])
        P.op("dve", lambda e: e.tensor_tensor(out=bst[:], in0=bs32[:], in1=bst[:], op=ALU.subtract), reads=["bs32", "bst"], writes=["bst2"])
        P.op("act", lambda e: e.copy(out=bsl[:], in_=bst[:]), reads=["bst2"], writes=["bsl"])
        uTr = Ring(k, "uT", 2, [128, 16, 128], BF16)
        vr = Ring(k, "vv", 2, [128, 2048], F32)
        vnr = Ring(k, "vn16", 2, [128, 2048], BF16)
        uvr = Ring(k, "uvT", 2, [128, 16, 128], BF16)
        stv = Ring(k, "stv", 2, [128, 24], F32)
        mvv = Ring(k, "mvv", 2, [128, 4], F32)

        def body(j, hT, hTk):
            uT, uTk = uTr.next()
            for cb in range(4):
                pb, pk = k.bank()

                def mmu(e, pb=pb, cb=cb):
                    for q in range(4):
                        ct = cb * 4 + q
                        for kc in range(8):
                            ins = e.matmul(pb[:, q * 128:(q + 1) * 128], lhsT=win[:, kc, ct * 128:(ct + 1) * 128], rhs=hT[:, kc, :],
                                           start=(kc == 0), stop=(kc == 7))
                    return ins
                P.op("pe", mmu, reads=[hTk] + winkeys, writes=[pk])
                for q in range(4):
                    ct = cb * 4 + q
                    P.op("act", lambda e, pb=pb, q=q, ct=ct: e.activation(out=uT[:, ct, :], in_=pb[:, q * 128:(q + 1) * 128], func=AF.Gelu,
                                                                       bias=binT[:, ct:ct + 1], scale=1.0), reads=[pk, "binT"], writes=[(uTk, ct)])
            v, vk = vr.next()
            for cb in range(4):
                pb, pk = k.bank()

                def mmv(e, pb=pb, cb=cb):
                    for kc in range(8):
                        e.matmul(pb[:], lhsT=hT[:, kc, :], rhs=win[:, kc, 2048 + cb * 512:2048 + (cb + 1) * 512], start=(kc == 0), stop=False)
                    return e.matmul(pb[:], lhsT=C["onesb"][0:1, :], rhs=bvb[0:1, cb * 512:(cb + 1) * 512], start=False, stop=True)
                P.op("pe", mmv, reads=[hTk, "bvb", "onesb"] + winkeys, writes=[pk])
                P.op("act", lambda e, pb=pb, cb=cb: e.activation(out=v[:, cb * 512:(cb + 1) * 512], in_=pb[:], func=AF.Gelu), reads=[pk], writes=[(vk, cb)])
            st, stk = stv.next()
            mv, mvk = mvv.next()
            for cb in range(4):
                P.op("dve", lambda e, cb=cb: e.bn_stats(out=st[:, cb * 6:(cb + 1) * 6], in_=v[:, cb * 512:(cb + 1) * 512]), reads=[(vk, cb)], writes=[(stk, cb)])
            P.op("dve", lambda e: e.bn_aggr(out=mv[:, 0:2], in_=st[:, 0:24]), reads=[(stk, cb) for cb in range(4)], writes=[mvk])
            P.op("act", lambda e: e.activation(out=mv[:, 2:3], in_=mv[:, 1:2], func=AF.Sqrt, bias=C["epsc"][:, 0:1], scale=1.0), reads=[mvk, "epsc"], writes=[mvk + "s"])
            P.op("dve", lambda e: e.reciprocal(out=mv[:, 3:4], in_=mv[:, 2:3]), reads=[mvk + "s"], writes=[mvk + "r"])
            P.op("dve", lambda e: e.tensor_scalar(out=v[:], in0=v[:], scalar1=mv[:, 0:1], scalar2=mv[:, 3:4], op0=ALU.subtract, op1=ALU.mult),
                 reads=[(vk, cb) for cb in range(4)] + [mvk, mvk + "r"], writes=[vk + "n"])
            P.op("pool", lambda e: e.tensor_tensor(out=v[:], in0=v[:], in1=vng[:], op=ALU.mult), reads=[vk + "n", "vng"], writes=[vk + "g"])
            vn, vnk = vnr.next()
            P.op("pool", lambda e: e.tensor_tensor(out=vn[:], in0=v[:], in1=vnb[:], op=ALU.add), reads=[vk + "g", "vnb"], writes=[vnk])
            uv, uvk = uvr.next()
            for cb in range(4):
                pb, pk = k.bank()

                def mms(e, pb=pb, cb=cb):
                    for q in range(4):
                        ct = cb * 4 + q
                        hg = ct // 2
                        e.matmul(pb[:, q * 128:(q + 1) * 128], lhsT=vn[:, ct * 128:(ct + 1) * 128], rhs=wsT[:, hg, :], start=True, stop=False)
                        e.matmul(pb[:, q * 128:(q + 1) * 128], lhsT=C["onesb"][0:1, :], rhs=bsh[0:1, hg * 128:(hg + 1) * 128], start=False, stop=False)
                        ins = e.matmul(pb[:, q * 128:(q + 1) * 128], lhsT=C["onesb"][0:1, :], rhs=bsl[0:1, hg * 128:(hg + 1) * 128], start=False, stop=True)
                    return ins
                P.op("pe", mms, reads=[vnk, "wsT", "bsh", "bsl", "onesb"], writes=[pk])
                P.op("dve", lambda e, pb=pb, cb=cb: e.tensor_tensor(out=uv[:, cb * 4:(cb + 1) * 4, :].rearrange("p a b -> p (a b)"), in0=pb[:],
                                                                 in1=uT[:, cb * 4:(cb + 1) * 4, :].rearrange("p a b -> p (a b)"), op=ALU.mult),
                     reads=[pk] + [(uTk, cb * 4 + q) for q in range(4)], writes=[(uvk, cb)])
            P.dma("sp", lambda e, j=j: e.dma_start(out=UVT[j], in_=uv[:]), reads=[(uvk, cb) for cb in range(4)], writes=[("UVT", j)])
        stage_a(k, li, C, body)

    wo = k.sb("gwo", [128, 16, D], BF16)
    for hh in range(4):
        P.dma("pool", lambda e, hh=hh: e.dma_start(out=wo[:, hh * 4:(hh + 1) * 4, :],
                                                  in_=wo_in[hh * 512:(hh + 1) * 512, :].rearrange("(c p) n -> p c n", p=128)), writes=[("gwo", hh)])
    bo = k.sb("gbo", [1, D], BF16)
    P.dma("pool", lambda e: e.dma_start(out=bo[:], in_=bo_in), writes=["gbo"])
    state = {}

    def y_src(j):
        if "r" not in state:
            state["r"] = Ring(k, "uvl", 2, [128, 16, 128], BF16)
        uv, uvk = state["r"].next()
        P.dma("sp", lambda e: e.dma_start(out=uv[:], in_=UVT[j]), reads=[("UVT", j)], writes=[uvk])
        res = []
        for half in range(2):
            pb, pk = k.bank()

            def mm(e, pb=pb, half=half):
                for ct in range(16):
                    e.matmul(pb[:], lhsT=uv[:, ct, :], rhs=wo[:, ct, half * 512:(half + 1) * 512], start=(ct == 0), stop=False)
                return e.matmul(pb[:], lhsT=C["onesb"][0:1, :], rhs=bo[0:1, half * 512:(half + 1) * 512], start=False, stop=True)
            P.op("pe", mm, reads=[uvk, "gbo", "onesb"] + [("gwo", hh) for hh in range(4)], writes=[pk])
            res.append((pb, pk))
        return res
    return y_src
```

```python
import contextlib
import numpy as np
import concourse.bass as bass
import concourse.mybir as mybir
from concourse.bass_utils import run_bass_kernel_spmd

F32 = mybir.dt.float32
BF16 = mybir.dt.bfloat16
AF = mybir.ActivationFunctionType
ALU = mybir.AluOpType

ISSUERS = ("pe", "act", "dve", "pool", "sp")


class Op:
    __slots__ = ("eng", "fn", "is_dma", "waits", "signal", "sem", "semval", "idx")

    def __init__(self, eng, fn, is_dma):
        self.eng = eng
        self.fn = fn
        self.is_dma = is_dma
        self.waits = []
        self.signal = False
        self.sem = None
        self.semval = 0


class Prog:
    def __init__(self, nc, n_dma_sems=8):
        self.nc = nc
        self.streams = {e: [] for e in ISSUERS}
        self.last_w = {}
        self.readers = {}
        self.n_dma_sems = n_dma_sems
        self.final_ops = []
        self.fence = {}
        self.dma_ops = {e: [] for e in ISSUERS}

    def barrier(self):
        deps = []
        for e in ISSUERS:
            if self.streams[e]:
                deps.append(self.streams[e][-1])
            deps.extend(self.dma_ops[e][-self.n_dma_sems:])
        self.fence = {e: list(deps) for e in ISSUERS}

    def _add(self, eng, fn, reads, writes, is_dma):
        op = Op(eng, fn, is_dma)
        op.idx = len(self.streams[eng])
        deps = []
        for r in reads:
            w = self.last_w.get(r)
            if w is not None:
                deps.append(w)
        for wkey in writes:
            w = self.last_w.get(wkey)
            if w is not None:
                deps.append(w)
            deps.extend(self.readers.get(wkey, ()))
        fz = self.fence.pop(eng, None)
        if fz:
            deps.extend(fz)
        if is_dma:
            self.dma_ops[eng].append(op)
        seen = set()
        best = {}
        for d in deps:
            if id(d) in seen or d is op:
                continue
            seen.add(id(d))
            if d.is_dma:
                op.waits.append(d)
                continue
            if d.eng == eng == "pe" and not is_dma and not fz:
                continue
            b = best.get(d.eng)
            if b is None or d.idx > b.idx:
                best[d.eng] = d
        op.waits.extend(best.values())
        for r in reads:
            lst = self.readers.setdefault(r, [])
            if not is_dma:
                for i_, o_ in enumerate(lst):
                    if (not o_.is_dma) and o_.eng == eng:
                        lst[i_] = op
                        break
                else:
                    lst.append(op)
            else:
                lst.append(op)
        for wkey in writes:
            self.last_w[wkey] = op
            self.readers[wkey] = []
        self.streams[eng].append(op)
        return op

    def op(self, eng, fn, reads=(), writes=()):
        return self._add(eng, fn, reads, writes, False)

    def dma(self, eng, fn, reads=(), writes=(), final=False):
        op = self._add(eng, fn, reads, writes, True)
        if final:
            self.final_ops.append(op)
        return op

    def emit(self):
        nc = self.nc
        if self.final_ops:
            fin = Op("sp", None, False)
            fin.waits = list(self.final_ops)
            self.streams["sp"].append(fin)
        for e in ISSUERS:
            for op in self.streams[e]:
                for d in op.waits:
                    d.signal = True
        with contextlib.ExitStack() as st:
            csem = {e: st.enter_context(nc.semaphore(f"c_{e}")) for e in ISSUERS}
            dsem = {e: [st.enter_context(nc.semaphore(f"d_{e}{i}")) for i in range(self.n_dma_sems)]
                    for e in ("sp", "act", "pool")}
            extra = []
            for e in ISSUERS:
                cnt = 0
                dcnt = [0] * self.n_dma_sems
                rr = 0
                prev_on_sem = [None] * self.n_dma_sems
                for op in self.streams[e]:
                    if op.is_dma:
                        s = rr % self.n_dma_sems
                        rr += 1
                        dcnt[s] += 16
                        op.sem = dsem[e][s]
                        op.semval = dcnt[s]
                        p = prev_on_sem[s]
                        if p is not None:
                            op.waits.append(p)
                        prev_on_sem[s] = op
                    elif op.signal:
                        cnt += 1
                        if cnt > 12000:
                            cnt = 1
                            csem[e] = st.enter_context(nc.semaphore(f"c_{e}_{len(extra)}"))
                            extra.append(1)
                        op.sem = csem[e]
                        op.semval = cnt
            block = st.enter_context(nc.Block())
            engs = {"pe": block.tensor, "act": block.scalar, "dve": block.vector,
                    "pool": block.gpsimd, "sp": block.sync}

            def make(e):
                def body(eng):
                    known = {}
                    for op in self.streams[e]:
                        for d in op.waits:
                            k = id(d.sem)
                            if known.get(k, 0) >= d.semval:
                                continue
                            eng.wait_ge(d.sem, d.semval)
                            known[k] = d.semval
                        if op.fn is None:
                            continue
                        ins = op.fn(eng)
                        if op.is_dma:
                            ins.then_inc(op.sem, 16)
                        elif op.signal:
                            ins.then_inc(op.sem, 1)
                return body

            for e in ISSUERS:
                if self.streams[e]:
                    engs[e](make(e))


D = 1024
NLAT = 16384
NCTX = 256
NTOK = NLAT + NCTX
NSUB = NTOK // 128
DEPTH = 4
ALPHA = float((2 * DEPTH) ** 0.25)
LN_EPS = 1e-5
NE = 32


class StopBuild(Exception):
    pass


STOP = [99]


def checkpoint(n):
    return STOP[0] <= n


class Ring:
    def __init__(self, K, name, n, shape, dt, psum=False):
        self.t = [(K.ps if psum else K.sb)(f"{name}{i}", shape, dt) for i in range(n)]
        self.keys = [f"{name}{i}" for i in range(n)]
        self.i = -1

    def next(self):
        self.i = (self.i + 1) % len(self.t)
        return self.t[self.i], self.keys[self.i]


class K:
    def __init__(self, layers):
        self.layers = layers
        self.nc = bass.Bass("TRN2", target_bir_lowering=False)
        self.P = Prog(self.nc)
        self.root = contextlib.ExitStack()
        self.scope = self.root
        self.uid = 0
        self.inputs = {}

    def din(self, name, shape, dt=F32):
        if name in self.inputs:
            return self.inputs[name]
        ap = self.nc.dram_tensor(name, list(shape), dt, kind="ExternalInput").ap()
        self.inputs[name] = ap
        return ap

    def dscr(self, name, shape, dt=F32):
        return self.nc.dram_tensor(name, list(shape), dt, kind="Internal").ap()

    def sb(self, name, shape, dt=F32):
        self.uid += 1
        return self.scope.enter_context(self.nc.sbuf_tensor(f"{name}_{self.uid}", list(shape), dt))

    def ps(self, name, shape, dt=F32):
        self.uid += 1
        return self.scope.enter_context(self.nc.psum_tensor(f"{name}_{self.uid}", list(shape), dt))

    @contextlib.contextmanager
    def sub(self):
        old = self.scope
        with contextlib.ExitStack() as s:
            self.scope = s
            try:
                yield
            finally:
                self.scope = old
                self.P.barrier()

    def bank(self):
        return self.banks.next()

    def load(self, eng, out, in_, reads, writes):
        return self.P.dma(eng, lambda e: e.dma_start(out=out, in_=in_), reads=reads, writes=writes)

    def rowload(self, tile, key, vec_ap, rkey=None):
        n = vec_ap.shape[-1]
        self.P.dma("sp", lambda e: e.dma_start(out=tile, in_=vec_ap.to_broadcast([128, n])),
                   reads=[rkey] if rkey else [], writes=[key])


def build(layers, out_layer_dbg=False):
    k = K(layers)
    nc, P = k.nc, k.P
    x_in = k.din("x", [NLAT, D])
    ctx_in = k.din("ctx", [NCTX, D])
    cc_in = k.din("cvec", [2, D])
    ident_in = k.din("ident", [128, 128])
    out = nc.dram_tensor("out", [NLAT, D], F32, kind="ExternalOutput").ap()
    X = k.dscr("X", [NTOK, D])
    ROWS = k.dscr("ROWS", [12, 128, D])
    H2T = k.dscr("H2T", [NSUB, 128, 8, 128], BF16)
    GATES = k.dscr("GATES", [NSUB, 128, NE])

    with k.sub():
        k.banks = Ring(k, "bank", 5, [128, 512], F32, psum=True)
        k.accs = Ring(k, "accb", 2, [128, 512], F32, psum=True)
        tpb = k.ps("tpb", [128, 1024], BF16)
        ident = k.sb("ident", [128, 128], BF16)
        P.dma("pool", lambda e: e.dma_start(out=ident[:], in_=ident_in), writes=["ident"])
        ones32 = k.sb("ones32", [128, 128])
        P.op("pool", lambda e: e.memset(ones32[:], 1.0), writes=["ones32"])
        onesb = k.sb("onesb", [128, 128], BF16)
        P.op("pool", lambda e: e.memset(onesb[:], 1.0), writes=["onesb"])
        zeros = k.sb("zeros", [128, 128])
        P.op("pool", lambda e: e.memset(zeros[:], 0.0), writes=["zeros"])
        ident32 = k.sb("ident32", [128, 128])
        P.dma("sp", lambda e: e.dma_start(out=ident32[:], in_=ident_in), writes=["ident32"])
        gring = Ring(k, "gt", 2, [128, NE + 2], F32)
        epsc = k.sb("epsc", [128, 1])
        P.op("pool", lambda e: e.memset(epsc[:], LN_EPS), writes=["epsc"])
        for q in range(16):
            P.dma("sp", lambda e, q=q: e.dma_start(out=X[q * 1024:(q + 1) * 1024, :], in_=x_in[q * 1024:(q + 1) * 1024, :]), writes=[("Xinit", q)])
        P.op("sp", lambda e: e.nop(), reads=[("Xinit", q) for q in range(16)], writes=["X"])
        P.dma("sp", lambda e: e.dma_start(out=X[NLAT:NTOK, :], in_=ctx_in), writes=["Xc"])
        cT = k.sb("cT", [128, 2, 8])
        with nc.allow_non_contiguous_dma(reason="tiny"):
            P.dma("sp", lambda e: e.dma_start(out=cT[:], in_=cc_in.rearrange("w (kc p) -> p w kc", p=128), allow_slow_non_contiguous=True), writes=["cT"])
        cS = k.sb("cS", [128, 2, 8])
        P.op("act", lambda e: e.activation(out=cS[:], in_=cT[:], func=AF.Silu), reads=["cT"], writes=["cS"])
        LB = k.sb("LB", [128, 2, 8, 128])
        for w in range(2):
            for kc in range(8):
                P.op("act", lambda e, w=w, kc=kc: e.activation(out=LB[:, w, kc, :], in_=zeros[:], func=AF.Identity,
                                                           bias=cS[:, w, kc:kc + 1], scale=1.0),
                     reads=["cS", "zeros"], writes=[("LB", w, kc)])
        LBkeys = [("LB", w, kc) for w in range(2) for kc in range(8)]

        try:
          for li in layers:
            layer(k, li, dict(X=X, ROWS=ROWS, H2T=H2T, GATES=GATES, ident=ident, ones32=ones32, onesb=onesb,
                              zeros=zeros, epsc=epsc, ident32=ident32, gring=gring, LB=LB, LBkeys=LBkeys, tpb=tpb))
        except StopBuild:
            pass

        P.dma("sp", lambda e: e.dma_start(out=out, in_=X[0:NLAT, :]), reads=["X"], final=True)
        P.emit()
    return k


def ada_rows(k, li, C):
    nc, P = k.nc, k.P
    aw = k.din(f"ada_w{li}", [D, 6 * D])
    ab = k.din(f"ada_b{li}", [1, 6 * D])
    ROWS, LB, ones32 = C["ROWS"], C["LB"], C["ones32"]
    with k.sub():
        wr = Ring(k, "adaw", 2, [128, 8, 512], F32)
        br = Ring(k, "adab", 2, [1, 512], F32)
        orr = Ring(k, "adao", 3, [128, 512], F32)
        for ct in range(12):
            wt, wk = wr.next()
            bt, bk = br.next()
            P.dma("sp", lambda e, wt=wt, ct=ct: e.dma_start(
                out=wt[:], in_=aw[:, ct * 512:(ct + 1) * 512].rearrange("(kc p) f -> p kc f", p=128)), writes=[wk])
            P.dma("sp", lambda e, bt=bt, ct=ct: e.dma_start(out=bt[:], in_=ab[:, ct * 512:(ct + 1) * 512]), writes=[bk])
            j, half = ct // 2, ct % 2
            for w in range(2):
                pb, pk = k.bank()

                def mm(e, pb=pb, wt=wt, bt=bt, w=w):
                    for kc in range(8):
                        e.matmul(pb[:], lhsT=LB[:, w, kc, :], rhs=wt[:, kc, :], start=(kc == 0), stop=False)
                    return e.matmul(pb[:], lhsT=ones32[0:1, :], rhs=bt[0:1, :], start=False, stop=True)
                P.op("pe", mm, reads=[wk, bk, "ones32"] + C["LBkeys"], writes=[pk])
                ot, ok = orr.next()
                add = 1.0 if j in (1, 4) else 0.0
                P.op("dve", lambda e, ot=ot, pb=pb, add=add: e.tensor_scalar(
                    out=ot[:], in0=pb[:], scalar1=add, scalar2=None, op0=ALU.add), reads=[pk], writes=[ok])
                P.dma("sp", lambda e, ot=ot, w=w, j=j, half=half: e.dma_start(
                    out=ROWS[w * 6 + j, :, half * 512:(half + 1) * 512], in_=ot[:]), reads=[ok], writes=[("ROWS", w * 6 + j, half)])


def rows_keys(idx):
    return [("ROWS", idx, 0), ("ROWS", idx, 1)]


def layernorm(k, z, zk, out_t, out_k, g_t, gk, b_t, bk, st6r, mvr, C):
    P = k.P
    st6, sk = st6r.next()
    mv, mk = mvr.next()
    P.op("dve", lambda e: e.bn_stats(out=st6[:, 0:6], in_=z[:, 0:512]), reads=[zk], writes=[sk + "a"])
    P.op("dve", lambda e: e.bn_stats(out=st6[:, 6:12], in_=z[:, 512:1024]), reads=[zk], writes=[sk + "b"])
    P.op("dve", lambda e: e.bn_aggr(out=mv[:, 0:2], in_=st6[:, 0:12]), reads=[sk + "a", sk + "b"], writes=[mk])
    P.op("act", lambda e: e.activation(out=mv[:, 2:3], in_=mv[:, 1:2], func=AF.Sqrt, bias=C["epsc"][:, 0:1], scale=1.0),
         reads=[mk, "epsc"], writes=[mk + "s"])
    P.op("dve", lambda e: e.reciprocal(out=mv[:, 3:4], in_=mv[:, 2:3]), reads=[mk + "s"], writes=[mk + "r"])
    P.op("dve", lambda e: e.tensor_scalar(out=out_t[:], in0=z[:], scalar1=mv[:, 0:1], scalar2=mv[:, 3:4],
                                          op0=ALU.subtract, op1=ALU.mult), reads=[zk, mk, mk + "r"], writes=[out_k])
    P.op("pool", lambda e: e.tensor_tensor(out=out_t[:], in0=out_t[:], in1=g_t[:], op=ALU.mult), reads=[out_k, gk], writes=[out_k])
    P.op("pool", lambda e: e.tensor_tensor(out=out_t[:], in0=out_t[:], in1=b_t[:], op=ALU.add), reads=[out_k, bk], writes=[out_k])


def modulate_T(k, xs, xk, scp, sck, sh, shk, hbr, hTr, C):
    P = k.P
    hb, hbk = hbr.next()
    hT, hTk = hTr.next()
    tpb, ident = C["tpb"], C["ident"]
    P.op("pool", lambda e: e.tensor_tensor(out=hb[0][:], in0=xs[:], in1=scp[:], op=ALU.mult), reads=[xk, sck], writes=[hbk + "f"])
    P.op("dve", lambda e: e.tensor_tensor(out=hb[1][:], in0=hb[0][:], in1=sh[:], op=ALU.add), reads=[hbk + "f", shk], writes=[hbk])

    def tr(e):
        for kc in range(8):
            ins = e.transpose(out=tpb[:, kc * 128:(kc + 1) * 128], in_=hb[1][:, kc * 128:(kc + 1) * 128], identity=ident[:])
        return ins
    P.op("pe", tr, reads=[hbk, "ident"], writes=["tpb"])
    P.op("act", lambda e: e.copy(out=hT[:].rearrange("p a b -> p (a b)"), in_=tpb[:]), reads=["tpb"], writes=[hTk])
    return hT, hTk


class PairRing:
    def __init__(self, k, name, n):
        self.t = [(k.sb(f"{name}f{i}", [128, D], F32), k.sb(f"{name}b{i}", [128, D], BF16)) for i in range(n)]
        self.keys = [f"{name}{i}" for i in range(n)]
        self.i = -1

    def next(self):
        self.i = (self.i + 1) % len(self.t)
        return self.t[self.i], self.keys[self.i]


def xrows(li, X, j):
    if li % 4 == 0 and j < 128:
        return X[0:NLAT, :].rearrange("(k2 k1) d -> k1 k2 d", k1=128)[j]
    return X[j * 128:(j + 1) * 128, :]


def layer(k, li, C):
    nc, P = k.nc, k.P
    kind = li % 4
    X, ROWS, H2T, GATES = C["X"], C["ROWS"], C["H2T"], C["GATES"]
    ada_rows(k, li, C)
    if checkpoint(1):
        return
    lnp = k.din(f"lnp{li}", [4, D])
    rw_in = k.din(f"router_w{li}", [D, NE])
    rb_in = k.din(f"router_b{li}", [1, NE])
    with_ctx = li < DEPTH - 1
    nsub_moe = NSUB if with_ctx else NSUB - 2

    with k.sub():
        if kind == 0:
            y_src = fourier_front(k, li, C)
        elif kind == 1:
            y_src = attn_front(k, li, C, dict(nkv=4, grp=4, p="fa", norm=True, sink=False, windowed=False))
        elif kind == 2:
            y_src = gmlp_front(k, li, C)
        else:
            y_src = attn_front(k, li, C, dict(nkv=2, grp=8, p="wa", norm=False, sink=True, windowed=True))
        if y_src is None:
            STOP[0] = 0

        with k.sub() if y_src is not None else contextlib.nullcontext():
            def row(name, src, rkeys=()):
                t = k.sb(name, [128, D])
                P.dma("sp", lambda e: e.dma_start(out=t[:], in_=src), reads=list(rkeys), writes=[name])
                return t
            g1 = [row(f"g1_{w}", ROWS[w * 6 + 2], rows_keys(w * 6 + 2)) for w in range(2)]
            sc2 = [row(f"sc2_{w}", ROWS[w * 6 + 4], rows_keys(w * 6 + 4)) for w in range(2)]
            sh2 = [row(f"sh2_{w}", ROWS[w * 6 + 3], rows_keys(w * 6 + 3)) for w in range(2)]
            lng = row("lng", lnp[0:1, :].to_broadcast([128, D]))
            lnb = row("lnb", lnp[1:2, :].to_broadcast([128, D]))
            rw = k.sb("rw", [128, 8, NE])
            with nc.allow_non_contiguous_dma(reason="small"):
                P.dma("sp", lambda e: e.dma_start(out=rw[:], in_=rw_in.rearrange("(kc p) e -> p kc e", p=128), allow_slow_non_contiguous=True), writes=["rw"])
            rb = k.sb("rb", [1, NE])
            P.dma("sp", lambda e: e.dma_start(out=rb[:], in_=rb_in), writes=["rb"])
            xr = Ring(k, "xs", 2, [128, D], F32)
            zr = Ring(k, "z", 2, [128, D], F32)
            x1r = Ring(k, "x1", 2, [128, D], F32)
            st6r = Ring(k, "st6", 2, [128, 12], F32)
            mvr = Ring(k, "mv", 2, [128, 4], F32)
            hbr = PairRing(k, "hb", 2)
            hTr = Ring(k, "hT", 2, [128, 8, 128], BF16)
            h32r = Ring(k, "h32", 2, [128, D], F32)
            hT32r = Ring(k, "hT32", 2, [128, 8, 128], F32)
            lgr = Ring(k, "lg", 2, [128, NE + 16], F32)
            for j in range(NSUB if y_src is not None else 0):
                w = 1 if j >= 128 else 0
                if w == 1 and not with_ctx:
                    continue
                xs, xk = xr.next()
                src = xrows(li, X, j)
                P.dma("sp", lambda e, xs=xs, src=src: e.dma_start(out=xs[:], in_=src), reads=["X", "Xc"], writes=[xk])
                z, zk = zr.next()
                y_halves = y_src(j)
                for hf in range(2):
                    pb, pk = y_halves[hf]
                    P.op("dve", lambda e, z=z, pb=pb, hf=hf, w=w: e.tensor_tensor(
                        out=z[:, hf * 512:(hf + 1) * 512], in0=pb[:], in1=g1[w][:, hf * 512:(hf + 1) * 512], op=ALU.mult),
                        reads=[pk, f"g1_{w}"], writes=[zk + str(hf)])
                P.op("dve", lambda e, z=z, xs=xs: e.scalar_tensor_tensor(
                    out=z[:], in0=xs[:], scalar=ALPHA, in1=z[:], op0=ALU.mult, op1=ALU.add),
                    reads=[xk, zk + "0", zk + "1"], writes=[zk])
                x1, x1k = x1r.next()
                layernorm(k, z, zk, x1, x1k, lng, "lng", lnb, "lnb", st6r, mvr, C)
                P.dma("sp", lambda e, x1=x1, src=src: e.dma_start(out=src, in_=x1[:]), reads=[x1k], writes=["X", "Xc"])
                hT, hTk = modulate_T(k, x1, x1k, sc2[w], f"sc2_{w}", sh2[w], f"sh2_{w}", hbr, hTr, C)
                P.dma("sp", lambda e, hT=hT, j=j: e.dma_start(out=H2T[j], in_=hT[:]), reads=[hTk], writes=[("H2T", j)])
                h32, h32k = h32r.next()
                P.op("pool", lambda e, h32=h32, x1=x1, w=w: e.tensor_tensor(out=h32[:], in0=x1[:], in1=sc2[w][:], op=ALU.mult),
                     reads=[x1k, f"sc2_{w}"], writes=[h32k + "a"])
                P.op("pool", lambda e, h32=h32, w=w: e.tensor_tensor(out=h32[:], in0=h32[:], in1=sh2[w][:], op=ALU.add),
                     reads=[h32k + "a", f"sh2_{w}"], writes=[h32k])
                hT32, hT32k = hT32r.next()
                for half in range(2):
                    pb, pk = k.bank()

                    def tr32(e, pb=pb, h32=h32, half=half):
                        for q in range(4):
                            kc = half * 4 + q
                            ins = e.matmul(pb[:, q * 128:(q + 1) * 128], lhsT=h32[:, kc * 128:(kc + 1) * 128], rhs=C["ident32"][:],
                                           start=True, stop=True)
                        return ins
                    P.op("pe", tr32, reads=[h32k, "ident32"], writes=[pk])
                    P.op("act", lambda e, pb=pb, hT32=hT32, half=half: e.copy(
                        out=hT32[:, half * 4:(half + 1) * 4, :].rearrange("p a b -> p (a b)"), in_=pb[:]), reads=[pk], writes=[hT32k + str(half)])
                pb, pk = k.bank()

                def rmm(e, pb=pb, hT32=hT32):
                    for kc in range(8):
                        e.matmul(pb[:, 0:NE], lhsT=hT32[:, kc, :], rhs=rw[:, kc, :], start=(kc == 0), stop=False)
                    return e.matmul(pb[:, 0:NE], lhsT=C["ones32"][0:1, :], rhs=rb[0:1, :], start=False, stop=True)
                P.op("pe", rmm, reads=[hT32k + "0", hT32k + "1", "rw", "rb", "ones32"], writes=[pk])
                lg, lgk = lgr.next()
                P.op("dve", lambda e, lg=lg, pb=pb: e.tensor_copy(out=lg[:, 0:NE], in_=pb[:, 0:NE]), reads=[pk], writes=[lgk + "l"])
                P.op("dve", lambda e, lg=lg: e.max(out=lg[:, NE:NE + 8], in_=lg[:, 0:NE]), reads=[lgk + "l"], writes=[lgk + "m"])
                P.op("dve", lambda e, lg=lg: e.tensor_scalar(out=lg[:, NE + 8:NE + 9], in0=lg[:, NE:NE + 1], scalar1=-1.0, scalar2=None,
                                                             op0=ALU.mult), reads=[lgk + "m"], writes=[lgk + "n"])
                gt, gtk = C["gring"].next()
                P.op("act", lambda e, lg=lg, gt=gt: e.activation(out=gt[:, 0:NE], in_=lg[:, 0:NE], func=AF.Exp,
                                                                bias=lg[:, NE + 8:NE + 9], scale=1.0), reads=[lgk + "l", lgk + "n"], writes=[gtk + "e"])
                P.op("dve", lambda e, lg=lg, gt=gt: e.scalar_tensor_tensor(
                    out=gt[:, 0:NE], in0=lg[:, 0:NE], scalar=lg[:, NE + 3:NE + 4], in1=gt[:, 0:NE], op0=ALU.is_ge, op1=ALU.mult),
                    reads=[lgk + "l", lgk + "m", gtk + "e"], writes=[gtk + "g"])
                P.op("dve", lambda e, gt=gt: e.tensor_reduce(out=gt[:, NE:NE + 1], in_=gt[:, 0:NE], axis=mybir.AxisListType.X, op=ALU.add),
                     reads=[gtk + "g"], writes=[gtk + "s"])
                P.op("dve", lambda e, gt=gt: e.reciprocal(out=gt[:, NE + 1:NE + 2], in_=gt[:, NE:NE + 1]), reads=[gtk + "s"], writes=[gtk + "r"])
                P.op("dve", lambda e, gt=gt: e.tensor_scalar(out=gt[:, 0:NE], in0=gt[:, 0:NE], scalar1=gt[:, NE + 1:NE + 2], scalar2=None,
                                                             op0=ALU.mult), reads=[gtk + "g", gtk + "r"], writes=[gtk])
                P.dma("sp", lambda e, gt=gt, j=j: e.dma_start(out=GATES[j], in_=gt[:, 0:NE]), reads=[gtk], writes=[("GATES", j)])

    if checkpoint(4):
        return
    moe(k, li, C, nsub_moe)


def fourier_front(k, li, C):
    nc, P = k.nc, k.P
    X, ROWS = C["X"], C["ROWS"]
    fw = k.din(f"fn_w{li}", [D, D])
    fb = k.din(f"fn_b{li}", [1, D])
    ccs_in = k.din("fn_ccs", [2, 256, 256])
    w128_in = k.din("fn_w128", [3, 128, 128])
    tw_in = k.din("fn_tw", [3, 128, 128])
    c8_in = k.din("fn_c8", [2, 256, 256])
    AB = k.dscr("fn_AB", [NTOK, 2 * D], BF16)
    VP = k.dscr("fn_VP", [128, 128, 2 * D], BF16)
    M12 = k.sb("M12", [128, 8, 2 * D], BF16)
    with k.sub():
        Wt = k.sb("fnW", [128, 8, D])
        P.dma("sp", lambda e: e.dma_start(out=Wt[:], in_=fw.rearrange("(kc p) n -> p kc n", p=128)), writes=["fnW"])
        ccs = k.sb("ccs", [128, 2, 2, 256])
        for t in range(2):
            P.dma("sp", lambda e, t=t: e.dma_start(out=ccs[:, t], in_=ccs_in[t].rearrange("(kk p) m -> p kk m", p=128)), writes=[("ccs", t)])
        for t in range(2):
            for rc in range(8):
                g = rc // 2
                for half in range(2):
                    pb, pk = k.bank()

                    def mm(e, pb=pb, t=t, rc=rc, g=g, half=half):
                        for kk in range(2):
                            ins = e.matmul(pb[:], lhsT=ccs[:, t, kk, (rc % 2) * 128:(rc % 2 + 1) * 128],
                                           rhs=Wt[:, g * 2 + kk, half * 512:(half + 1) * 512], start=(kk == 0), stop=(kk == 1))
                        return ins
                    P.op("pe", mm, reads=["fnW", ("ccs", t)], writes=[pk])
                    sc = (1.0 if t == 0 else -1.0) / 2048.0
                    P.op("dve", lambda e, pb=pb, t=t, rc=rc, half=half, sc=sc: e.tensor_scalar(
                        out=M12[:, rc, t * D + half * 512: t * D + (half + 1) * 512], in0=pb[:], scalar1=sc, scalar2=None, op0=ALU.mult),
                        reads=[pk], writes=[("M12", t, rc, half)])
    M12keys = [("M12", t, rc, half) for t in range(2) for rc in range(8) for half in range(2)]
    if checkpoint(1.5):
        return None

    with k.sub():
        def row(name, src, rkeys):
            t = k.sb(name, [128, D])
            P.dma("sp", lambda e: e.dma_start(out=t[:], in_=src), reads=list(rkeys), writes=[name])
            return t
        sc1 = [row(f"sc1_{w}", ROWS[w * 6 + 1], rows_keys(w * 6 + 1)) for w in range(2)]
        sh1 = [row(f"sh1_{w}", ROWS[w * 6 + 0], rows_keys(w * 6 + 0)) for w in range(2)]
        xr = Ring(k, "xsA", 3, [128, D], F32)
        hbr = PairRing(k, "hbA", 2)
        hTr = Ring(k, "hTA", 2, [128, 8, 128], BF16)
        abr = Ring(k, "abA", 2, [128, 2 * D], BF16)
        import os
        for j in range(int(os.environ.get("NSUB_A", NSUB))):
            w = 1 if j >= 128 else 0
            xs, xk = xr.next()
            P.dma("sp", lambda e, xs=xs, j=j: e.dma_start(out=xs[:], in_=X[j * 128:(j + 1) * 128, :]), reads=["X", "Xc"], writes=[xk])
            hT, hTk = modulate_T(k, xs, xk, sc1[w], f"sc1_{w}", sh1[w], f"sh1_{w}", hbr, hTr, C)
            ab, abk = abr.next()
            for ct in range(4):
                pb, pk = k.bank()

                def mm(e, pb=pb, hT=hT, ct=ct):
                    for kc in range(8):
                        ins = e.matmul(pb[:], lhsT=hT[:, kc, :], rhs=M12[:, kc, ct * 512:(ct + 1) * 512], start=(kc == 0), stop=(kc == 7))
                    return ins
                P.op("pe", mm, reads=[hTk] + M12keys, writes=[pk])
                if ct % 2 == 0:
                    P.op("act", lambda e, pb=pb, ab=ab, ct=ct: e.copy(out=ab[:, ct * 512:(ct + 1) * 512], in_=pb[:]), reads=[pk], writes=[abk + str(ct)])
                else:
                    P.op("dve", lambda e, pb=pb, ab=ab, ct=ct: e.tensor_copy(out=ab[:, ct * 512:(ct + 1) * 512], in_=pb[:]), reads=[pk], writes=[abk + str(ct)])
            P.dma("sp", lambda e, ab=ab, j=j: e.dma_start(out=AB[j * 128:(j + 1) * 128, :], in_=ab[:]),
                  reads=[abk + str(ct) for ct in range(4)], writes=[("AB", j)])
    ABkeys = [("AB", j) for j in range(128)]
    if checkpoint(2):
        return None

    w128 = k.sb("w128", [128, 3, 128], BF16)
    P.dma("pool", lambda e: e.dma_start(out=w128[:], in_=w128_in.rearrange("t p m -> p t m")), writes=["w128"])
    tw = k.sb("tw", [128, 3, 128])
    P.dma("sp", lambda e: e.dma_start(out=tw[:], in_=tw_in.rearrange("t p m -> p t m")), writes=["tw"])
    c8 = k.sb("c8", [128, 2, 2, 256], BF16)
    for t in range(2):
        P.dma("pool", lambda e, t=t: e.dma_start(out=c8[:, t], in_=c8_in[t].rearrange("(kk p) m -> p kk m", p=128)), writes=[("c8", t)])
    fbb = k.sb("fbb", [1, D], BF16)
    P.dma("pool", lambda e: e.dma_start(out=fbb[:], in_=fb), writes=["fbb"])

    with k.sub():
        ur = Ring(k, "u1", 2, [128, 2 * D], BF16)
        vr = Ring(k, "v1", 2, [128, 2 * D], BF16)
        tr_ = Ring(k, "t1", 8, [128, 512], F32)
        ABv = AB[0:NLAT, :].rearrange("(n1 n2) c -> n2 n1 c", n2=128)
        import os
        for n2 in range(int(os.environ.get("NSUB_1", 128))):
            u, uk = ur.next()
            P.dma("sp", lambda e, u=u, n2=n2: e.dma_start(out=u[:], in_=ABv[n2]), reads=ABkeys, writes=[uk])
            v, vk = vr.next()
            for half in range(2):
                sl = slice(half * 512, (half + 1) * 512)
                sli = slice(D + half * 512, D + (half + 1) * 512)
                pr, prk = k.bank()
                pi, pik = k.bank()

                def mmr(e, pr=pr, u=u, sl=sl, sli=sli):
                    e.matmul(pr[:], lhsT=w128[:, 0, :], rhs=u[:, sl], start=True, stop=False)
                    return e.matmul(pr[:], lhsT=w128[:, 1, :], rhs=u[:, sli], start=False, stop=True)

                def mmi(e, pi=pi, u=u, sl=sl, sli=sli):
                    e.matmul(pi[:], lhsT=w128[:, 0, :], rhs=u[:, sli], start=True, stop=False)
                    return e.matmul(pi[:], lhsT=w128[:, 2, :], rhs=u[:, sl], start=False, stop=True)
                P.op("pe", mmr, reads=[uk, "w128"], writes=[prk])
                P.op("pe", mmi, reads=[uk, "w128"], writes=[pik])
                t1, t1k = tr_.next()
                t2, t2k = tr_.next()
                t3, t3k = tr_.next()
                t4, t4k = tr_.next()
                for (tt, ttk, src, srck, col) in ((t1, t1k, pr, prk, 0), (t3, t3k, pi, pik, 1), (t2, t2k, pi, pik, 0), (t4, t4k, pr, prk, 2)):
                    P.op("dve", lambda e, tt=tt, src=src, col=col, n2=n2: e.tensor_scalar(
                        out=tt[:], in0=src[:], scalar1=tw[:, col, n2:n2 + 1], scalar2=None, op0=ALU.mult), reads=[srck, "tw"], writes=[ttk])
                P.op("pool", lambda e, v=v, t1=t1, t3=t3, sl=sl: e.tensor_tensor(out=v[:, sl], in0=t1[:], in1=t3[:], op=ALU.add),
                     reads=[t1k, t3k], writes=[vk + "r" + str(half)])
                P.op("pool", lambda e, v=v, t2=t2, t4=t4, sli=sli: e.tensor_tensor(out=v[:, sli], in0=t2[:], in1=t4[:], op=ALU.add),
                     reads=[t2k, t4k], writes=[vk + "i" + str(half)])
            P.dma("sp", lambda e, v=v, n2=n2: e.dma_start(out=VP[:, n2, :], in_=v[:]),
                  reads=[vk + a + str(h) for a in "ri" for h in range(2)], writes=[("VP", n2)])
    VPkeys = [("VP", n2) for n2 in range(128)]
    if checkpoint(3):
        return None

    state = {}

    def y_src(j):
        if "vpr" not in state:
            state["vpr"] = Ring(k, "vp2", 2, [128, 2 * D], BF16)
            state["abc"] = k.sb("abctx", [128, 2, 2 * D], BF16)
            P.dma("sp", lambda e: e.dma_start(out=state["abc"][:], in_=AB[NLAT:NTOK, :].rearrange("(kk p) c -> p kk c", p=128)),
                  reads=[("AB", 128), ("AB", 129)], writes=["abctx"])
        res = []
        if j < 128:
            vp, vpk = state["vpr"].next()
            P.dma("sp", lambda e: e.dma_start(out=vp[:], in_=VP[j]), reads=VPkeys, writes=[vpk])
            for half in range(2):
                pb, pk = k.bank()

                def mm(e, pb=pb, half=half):
                    e.matmul(pb[:], lhsT=w128[:, 0, :], rhs=vp[:, half * 512:(half + 1) * 512], start=True, stop=False)
                    e.matmul(pb[:], lhsT=w128[:, 1, :], rhs=vp[:, D + half * 512:D + (half + 1) * 512], start=False, stop=False)
                    return e.matmul(pb[:], lhsT=C["onesb"][0:1, :], rhs=fbb[0:1, half * 512:(half + 1) * 512], start=False, stop=True)
                P.op("pe", mm, reads=[vpk, "w128", "onesb", "fbb"], writes=[pk])
                res.append((pb, pk))
        else:
            kt = j - 128
            abc = state["abc"]
            for half in range(2):
                pb, pk = k.bank()

                def mm(e, pb=pb, half=half):
                    for kk in range(2):
                        e.matmul(pb[:], lhsT=c8[:, 0, kk, kt * 128:(kt + 1) * 128], rhs=abc[:, kk, half * 512:(half + 1) * 512],
                                 start=(kk == 0), stop=False)
                        e.matmul(pb[:], lhsT=c8[:, 1, kk, kt * 128:(kt + 1) * 128], rhs=abc[:, kk, D + half * 512:D + (half + 1) * 512],
                                 start=False, stop=False)
                    return e.matmul(pb[:], lhsT=C["onesb"][0:1, :], rhs=fbb[0:1, half * 512:(half + 1) * 512], start=False, stop=True)
                P.op("pe", mm, reads=["abctx", ("c8", 0), ("c8", 1), "onesb", "fbb"], writes=[pk])
                res.append((pb, pk))
        return res
    return y_src


def moe(k, li, C, nsub):
    nc, P = k.nc, k.P
    X, ROWS, H2T, GATES = C["X"], C["ROWS"], C["H2T"], C["GATES"]
    wgu_in = k.din(f"wgu{li}", [NE, D, 2 * D])
    bgu_in = k.din(f"bgu{li}", [NE, 2 * D])
    wd_in = k.din(f"wd{li}", [NE, D, D])
    bd_in = k.din(f"bd{li}", [NE, D])
    lnp = k.inputs[f"lnp{li}"]
    GS = 4
    with k.sub():
        def row(name, src, rkeys=()):
            t = k.sb(name, [128, D])
            P.dma("sp", lambda e: e.dma_start(out=t[:], in_=src), reads=list(rkeys), writes=[name])
            return t
        g2 = [row(f"g2_{w}", ROWS[w * 6 + 5], rows_keys(w * 6 + 5)) for w in range(2)]
        lng = row("lnfg", lnp[2:3, :].to_broadcast([128, D]))
        lnb = row("lnfb", lnp[3:4, :].to_broadcast([128, D]))
        bgu = k.sb("bgu", [128, NE, 8, 2])
        with nc.allow_non_contiguous_dma(reason="bias layout"):
            for e_ in range(NE):
                P.dma("sp", lambda e, e_=e_: e.dma_start(out=bgu[:, e_], in_=bgu_in[e_].rearrange("(c p j) -> p c j", p=128, j=2), allow_slow_non_contiguous=True),
                      writes=[("bgu", e_)])
        hTg = k.sb("hTg", [128, 8, GS, 128], BF16)
        Gg = k.sb("Gg", [128, GS, NE])
        acc = k.sb("acc", [128, GS, D])
        act = k.sb("actm", [128, 8, GS * 128], BF16)
        wgur = Ring(k, "wgu", 2, [128, 8, 2 * D], BF16)
        wdr = Ring(k, "wd", 2, [128, 8, D], BF16)
        bdr = Ring(k, "bd", 2, [1, D], BF16)
        tgr = Ring(k, "tg", 2, [128, 512], F32)
        tsr = Ring(k, "tsg", 2, [128, 512], F32)
        tlr = Ring(k, "tl", 2, [128, 512], F32)
        xr = Ring(k, "xsD", 2, [128, D], F32)
        zr = Ring(k, "zD", 2, [128, D], F32)
        x2r = Ring(k, "x2", 2, [128, D], F32)
        st6r = Ring(k, "st6D", 2, [128, 12], F32)
        mvr = Ring(k, "mvD", 2, [128, 4], F32)
        ngroups = (nsub + GS - 1) // GS
        pend = [None]

        def issue_load(ex):
            wgu, wguk = wgur.next()
            wd, wdk = wdr.next()
            bd, bdk = bdr.next()
            for hh in range(2):
                P.dma("pool", lambda e, wgu=wgu, ex=ex, hh=hh: e.dma_start(
                    out=wgu[:, hh * 4:(hh + 1) * 4, :], in_=wgu_in[ex, hh * 512:(hh + 1) * 512, :].rearrange("(kc p) f -> p kc f", p=128)),
                    writes=[wguk + str(hh)])
            P.dma("pool", lambda e, wd=wd, ex=ex: e.dma_start(out=wd[:], in_=wd_in[ex].rearrange("(kc p) f -> p kc f", p=128)), writes=[wdk])
            P.dma("pool", lambda e, bd=bd, ex=ex: e.dma_start(out=bd[:], in_=bd_in[ex:ex + 1, :]), writes=[bdk])
            return (wgu, wguk, wd, wdk, bd, bdk)
        for g in range(ngroups):
            j0 = g * GS
            ns = min(GS, nsub - j0)
            ntok = ns * 128
            for s_ in range(ns):
                P.dma("sp", lambda e, j0=j0, s_=s_: e.dma_start(out=hTg[:, :, s_, :], in_=H2T[j0 + s_]),
                      reads=[("H2T", j0 + s_)], writes=[("hTg", s_)])
            P.dma("sp", lambda e, j0=j0, ns=ns: e.dma_start(out=Gg[:, 0:ns, :], in_=GATES[j0:j0 + ns].rearrange("s p e -> p s e")),
                  reads=[("GATES", j) for j in range(j0, j0 + ns)], writes=["Gg"])
            P.op("pool", lambda e: e.memset(acc[:], 0.0), writes=[("acc", s, h) for s in range(GS) for h in range(2)])
            tchunks = [(t0, min(512, ntok - t0)) for t0 in range(0, ntok, 512)]
            for ex in range(NE):
                if pend[0] is None:
                    pend[0] = issue_load(ex)
                wgu, wguk, wd, wdk, bd, bdk = pend[0]
                pend[0] = None
                if not (g == ngroups - 1 and ex == NE - 1):
                    pend[0] = issue_load((ex + 1) % NE)
                for c in range(8):
                    for (t0, tn) in tchunks:
                        pg, pgk = k.bank()
                        pl, plk = k.bank()
                        rhs_of = lambda kc, t0=t0, tn=tn: hTg[:, kc, t0 // 128:(t0 + tn) // 128, :].rearrange("p a b -> p (a b)")

                        def mmg(e, pg=pg, wgu=wgu, c=c, tn=tn, rhs_of=rhs_of):
                            for kc in range(8):
                                ins = e.matmul(pg[:, 0:tn], lhsT=wgu[:, kc, c * 256:(c + 1) * 256:2], rhs=rhs_of(kc), start=(kc == 0), stop=(kc == 7))
                            return ins

                        def mml(e, pl=pl, wgu=wgu, c=c, tn=tn, rhs_of=rhs_of):
                            for kc in range(8):
                                ins = e.matmul(pl[:, 0:tn], lhsT=wgu[:, kc, c * 256 + 1:(c + 1) * 256:2], rhs=rhs_of(kc), start=(kc == 0), stop=(kc == 7))
                            return ins
                        P.op("pe", mmg, reads=[wguk + "0", wguk + "1"] + [("hTg", s_) for s_ in range(ns)], writes=[pgk])
                        P.op("pe", mml, reads=[wguk + "0", wguk + "1"] + [("hTg", s_) for s_ in range(ns)], writes=[plk])
                        tg, tgk = tgr.next()
                        ts, tsk = tsr.next()
                        tl, tlk = tlr.next()
                        P.op("dve", lambda e, tg=tg, pg=pg, tn=tn, ex=ex, c=c: e.tensor_scalar(
                            out=tg[:, 0:tn], in0=pg[:, 0:tn], scalar1=bgu[:, ex, c, 0:1], scalar2=7.0, op0=ALU.add, op1=ALU.min),
                            reads=[pgk, ("bgu", ex)], writes=[tgk])
                        P.op("act", lambda e, ts=ts, tg=tg, tn=tn: e.activation(out=ts[:, 0:tn], in_=tg[:, 0:tn], func=AF.Sigmoid, scale=1.702),
                             reads=[tgk], writes=[tsk])
                        P.op("dve", lambda e, tl=tl, pl=pl, tn=tn, ex=ex, c=c: e.tensor_scalar(
                            out=tl[:, 0:tn], in0=pl[:, 0:tn], scalar1=bgu[:, ex, c, 1:2], scalar2=7.0, op0=ALU.add, op1=ALU.min),
                            reads=[plk, ("bgu", ex)], writes=[tlk])
                        P.op("pool", lambda e, tl=tl, tn=tn: e.tensor_scalar(
                            out=tl[:, 0:tn], in0=tl[:, 0:tn], scalar1=-7.0, scalar2=1.0, op0=ALU.max, op1=ALU.add), reads=[tlk], writes=[tlk])
                        P.op("pool", lambda e, tg=tg, ts=ts, tn=tn: e.tensor_tensor(out=tg[:, 0:tn], in0=tg[:, 0:tn], in1=ts[:, 0:tn], op=ALU.mult),
                             reads=[tgk, tsk], writes=[tgk])
                        P.op("dve", lambda e, tg=tg, tl=tl, c=c, t0=t0, tn=tn: e.tensor_tensor(
                            out=act[:, c, t0:t0 + tn], in0=tg[:, 0:tn], in1=tl[:, 0:tn], op=ALU.mult), reads=[tgk, tlk], writes=[("act", c, t0)])
                actkeys = [("act", c, t0) for c in range(8) for (t0, tn) in tchunks]
                for s in range(ns):
                    for half in range(2):
                        pb, pk = k.bank()

                        def mmd(e, pb=pb, wd=wd, bd=bd, s=s, half=half):
                            for fc in range(8):
                                e.matmul(pb[:], lhsT=act[:, fc, s * 128:(s + 1) * 128], rhs=wd[:, fc, half * 512:(half + 1) * 512], start=(fc == 0), stop=False)
                            return e.matmul(pb[:], lhsT=C["onesb"][0:1, :], rhs=bd[0:1, half * 512:(half + 1) * 512], start=False, stop=True)
                        P.op("pe", mmd, reads=actkeys + [wdk, bdk, "onesb"], writes=[pk])
                        tq, tqk = tgr.next()
                        P.op("dve", lambda e, pb=pb, s=s, ex=ex, tq=tq: e.tensor_scalar(
                            out=tq[:], in0=pb[:], scalar1=Gg[:, s, ex:ex + 1], scalar2=None, op0=ALU.mult), reads=[pk, "Gg"], writes=[tqk])
                        P.op("pool", lambda e, s=s, half=half, tq=tq: e.tensor_tensor(
                            out=acc[:, s, half * 512:(half + 1) * 512], in0=acc[:, s, half * 512:(half + 1) * 512], in1=tq[:], op=ALU.add),
                            reads=[tqk, ("acc", s, half)], writes=[("acc", s, half)])
            for s in range(ns):
                j = j0 + s
                w = 1 if j >= 128 else 0
                xs, xk = xr.next()
                src = xrows(li, X, j)
                P.dma("sp", lambda e, xs=xs, src=src: e.dma_start(out=xs[:], in_=src), reads=["X", "Xc"], writes=[xk])
                z, zk = zr.next()
                P.op("dve", lambda e, z=z, s=s, w=w: e.tensor_tensor(out=z[:], in0=acc[:, s, :], in1=g2[w][:], op=ALU.mult),
                     reads=[("acc", s, 0), ("acc", s, 1), f"g2_{w}"], writes=[zk + "a"])
                P.op("dve", lambda e, z=z, xs=xs: e.scalar_tensor_tensor(out=z[:], in0=xs[:], scalar=ALPHA, in1=z[:], op0=ALU.mult, op1=ALU.add),
                     reads=[xk, zk + "a"], writes=[zk])
                x2, x2k = x2r.next()
                layernorm(k, z, zk, x2, x2k, lng, "lnfg", lnb, "lnfb", st6r, mvr, C)
                P.dma("sp", lambda e, x2=x2, src=src: e.dma_start(out=src, in_=x2[:]), reads=[x2k], writes=["X", "Xc"])


def _tables():
    t = {}
    i256 = np.arange(256)
    ang = 2 * np.pi * np.outer(i256, i256) / 256.0
    t["fn_ccs"] = np.stack([np.cos(ang), np.sin(ang)]).astype(np.float32)
    t["fn_c8"] = (8.0 * np.stack([np.cos(ang), np.sin(ang)])).astype(np.float32)
    i128 = np.arange(128)
    a128 = 2 * np.pi * np.outer(i128, i128) / 128.0
    t["fn_w128"] = np.stack([np.cos(a128), np.sin(a128), -np.sin(a128)]).astype(np.float32)
    atw = 2 * np.pi * np.outer(i128, i128) / 16384.0
    t["fn_tw"] = np.stack([np.cos(atw), np.sin(atw), -np.sin(atw)]).astype(np.float32)
    t["ident"] = np.eye(128, dtype=np.float32)
    rows = NLAT // 64
    row = np.repeat(np.arange(rows, dtype=np.float32), 64)
    col = np.tile(np.arange(64, dtype=np.float32), rows)
    inv = (np.float32(10000.0) ** (-np.arange(16, dtype=np.float32) / np.float32(16))).astype(np.float32)
    ang = np.concatenate([row[:, None] * inv, col[:, None] * inv], axis=-1).astype(np.float32)
    t["rope"] = np.stack([np.tile(np.cos(ang), (1, 16)), np.tile(np.sin(ang), (1, 16))]).astype(np.float32)
    kk = np.arange(128)[:, None]
    qq = np.arange(128)[None, :]
    t["wa_mask"] = np.stack([np.tile((kk >= qq), (1, 4)), np.tile((kk <= qq), (1, 4))]).astype(np.float32)
    return t


_CACHE = {}


def kernel(**inp):
    layers = list(range(DEPTH))
    if "k" not in _CACHE:
        _CACHE["k"] = build(layers)
    k = _CACHE["k"]
    f = lambda a: np.ascontiguousarray(np.asarray(a, dtype=np.float32))
    m = dict(_tables())
    m["x"] = f(inp["x"]).reshape(NLAT, D)
    m["ctx"] = f(inp["ctx"]).reshape(NCTX, D)
    m["cvec"] = np.stack([f(inp["c"]).reshape(D), f(inp["c_ctx"]).reshape(D)])
    for li in layers:
        m[f"ada_w{li}"] = f(inp["ada_w"][li])
        m[f"ada_b{li}"] = f(inp["ada_b"][li]).reshape(1, -1)
        m[f"lnp{li}"] = np.stack([f(inp["ln_mix_g"][li]), f(inp["ln_mix_b"][li]), f(inp["ln_ffn_g"][li]), f(inp["ln_ffn_b"][li])])
        m[f"router_w{li}"] = f(inp["router_w"][li])
        m[f"router_b{li}"] = f(inp["router_b"][li]).reshape(1, -1)
        m[f"wgu{li}"] = f(inp["exp_w_gate_up"][li])
        m[f"bgu{li}"] = f(inp["exp_b_gate_up"][li])
        m[f"wd{li}"] = f(inp["exp_w_down"][li])
        m[f"bd{li}"] = f(inp["exp_b_down"][li])
        jj = li // 4
        if li % 4 == 0:
            m[f"fn_w{li}"] = f(inp["fn_w_out"][jj])
            m[f"fn_b{li}"] = f(inp["fn_b_out"][jj]).reshape(1, -1)
        elif li % 4 == 1:
            m[f"fa_wqkv{li}"] = f(inp["fa_w_qkv"][jj])
            m[f"fa_bqkv{li}"] = f(inp["fa_b_qkv"][jj]).reshape(1, -1)
            m[f"fa_wo{li}"] = f(inp["fa_w_out"][jj])
            m[f"fa_bo{li}"] = f(inp["fa_b_out"][jj]).reshape(1, -1)
            m[f"fa_qn{li}"] = np.tile(f(inp["fa_q_norm"][jj]), 16).reshape(1, -1)
            m[f"fa_kn{li}"] = np.tile(f(inp["fa_k_norm"][jj]), 4).reshape(1, -1)
        elif li % 4 == 2:
            m[f"gm_win{li}"] = f(inp["gm_w_in"][jj])
            m[f"gm_bin{li}"] = f(inp["gm_b_in"][jj]).reshape(1, -1)
            m[f"gm_vng{li}"] = f(inp["gm_v_norm_g"][jj]).reshape(1, -1)
            m[f"gm_vnb{li}"] = f(inp["gm_v_norm_b"][jj]).reshape(1, -1)
            m[f"gm_wsT{li}"] = f(np.transpose(np.asarray(inp["gm_w_s"][jj]), (0, 2, 1)))
            m[f"gm_bs{li}"] = f(inp["gm_b_s"][jj]).reshape(1, -1)
            m[f"gm_wo{li}"] = f(inp["gm_w_out"][jj])
            m[f"gm_bo{li}"] = f(inp["gm_b_out"][jj]).reshape(1, -1)
        else:
            m[f"wa_wqkv{li}"] = f(inp["wa_w_qkv"][jj])
            m[f"wa_bqkv{li}"] = f(inp["wa_b_qkv"][jj]).reshape(1, -1)
            m[f"wa_wo{li}"] = f(inp["wa_w_out"][jj])
            m[f"wa_bo{li}"] = f(inp["wa_b_out"][jj]).reshape(1, -1)
            m[f"wa_sink{li}"] = np.repeat(f(inp["wa_sink"][jj]), 128).reshape(1, -1)
    m = {n: m[n] for n in k.inputs}
    res = run_bass_kernel_spmd(k.nc, [m], core_ids=[0])
    return np.asarray(res.results[0]["out"], dtype=np.float32).reshape(1, NLAT, D)


def stage_a(k, li, C, body):
    P = k.P
    X, ROWS = C["X"], C["ROWS"]
    sc1 = k.sb("sc1", [128, D])
    sh1 = k.sb("sh1", [128, D])

    def load_rows(w):
        P.dma("sp", lambda e: e.dma_start(out=sc1[:], in_=ROWS[w * 6 + 1]), reads=rows_keys(w * 6 + 1), writes=["sc1"])
        P.dma("sp", lambda e: e.dma_start(out=sh1[:], in_=ROWS[w * 6 + 0]), reads=rows_keys(w * 6 + 0), writes=["sh1"])
    xr = Ring(k, "xsA", 3, [128, D], F32)
    hbr = PairRing(k, "hbA", 2)
    hTr = Ring(k, "hTA", 2, [128, 8, 128], BF16)
    import os
    for j in (list(range(NSUB)) if "NSUB_A" not in os.environ else [0, 129][:int(os.environ["NSUB_A"])]):
        if j == 0:
            load_rows(0)
        if j == 128:
            load_rows(1)
        xs, xk = xr.next()
        P.dma("sp", lambda e, xs=xs, j=j: e.dma_start(out=xs[:], in_=X[j * 128:(j + 1) * 128, :]), reads=["X", "Xc"], writes=[xk])
        hT, hTk = modulate_T(k, xs, xk, sc1, "sc1", sh1, "sh1", hbr, hTr, C)
        body(j, hT, hTk)


def attn_front(k, li, C, cfg):
    nc, P = k.nc, k.P
    nkv, grp, p = cfg["nkv"], cfg["grp"], cfg["p"]
    norm, sink, windowed = cfg["norm"], cfg["sink"], cfg["windowed"]
    with_ctx = li < DEPTH - 1
    KW = nkv * 64
    QKVW = D + 2 * KW
    wq_in = k.din(f"{p}_wqkv{li}", [D, QKVW])
    bq_in = k.din(f"{p}_bqkv{li}", [1, QKVW])
    wo_in = k.din(f"{p}_wo{li}", [D, D])
    bo_in = k.din(f"{p}_bo{li}", [1, D])
    if "rope_in" not in C:
        C["rope_in"] = k.din("rope", [2, NLAT, 512])
    rope_in = C["rope_in"]
    if norm:
        qn_in = k.din(f"{p}_qn{li}", [1, D])
        kn_in = k.din(f"{p}_kn{li}", [1, KW])
    if sink:
        sink_in = k.din(f"{p}_sink{li}", [1, 2048])
    if windowed:
        mask_in = k.din(f"{p}_mask", [2, 128, 512])
    Q = k.dscr(f"{p}_Q{li}", [NTOK, D], BF16)
    KT = k.dscr(f"{p}_KT{li}", [nkv, 64, NTOK], BF16)
    V = k.dscr(f"{p}_V{li}", [NTOK, KW], BF16)
    OT = k.dscr(f"{p}_OT{li}", [NSUB, 64, 16, 128], BF16)
    tpb, ident = C["tpb"], C["ident"]

    with k.sub():
        wq = k.sb("wq", [128, 8, QKVW], BF16)
        for hh in range(2):
            P.dma("pool", lambda e, hh=hh: e.dma_start(out=wq[:, hh * 4:(hh + 1) * 4, :],
                                                      in_=wq_in[hh * 512:(hh + 1) * 512, :].rearrange("(kc p) f -> p kc f", p=128)),
                  writes=[("wq", hh)])
        bq = k.sb("bq", [1, QKVW], BF16)
        P.dma("pool", lambda e: e.dma_start(out=bq[:], in_=bq_in), writes=["bq"])
        if norm:
            gq = k.sb("gq", [128, D])
            P.dma("sp", lambda e: e.dma_start(out=gq[:], in_=qn_in.to_broadcast([128, D])), writes=["gq"])
            gk = k.sb("gk", [128, KW])
            P.dma("sp", lambda e: e.dma_start(out=gk[:], in_=kn_in.to_broadcast([128, KW])), writes=["gk"])
        NH = 16 + nkv
        qkr = Ring(k, "qkraw", 2, [128, D + KW], F32)
        qkn = Ring(k, "qkn", 2, [128, D + KW], F32)
        sqr = Ring(k, "sq", 1, [128, D + KW], F32)
        ssr = Ring(k, "ss", 2, [128, 2 * NH], F32)
        vtr = Ring(k, "vt", 2, [128, KW], BF16)
        qrr = Ring(k, "qr", 2, [128, D + KW], BF16)
        csr = Ring(k, "cs", 2, [128, 2, 512], F32)
        tmr = Ring(k, "ropet", 4, [128, (D + KW) // 2], F32)
        ktr = Ring(k, "kTt", 2, [64, nkv * 128], BF16)
        HP = (D + KW) // 2
        colsets = [(0, 512), (512, 1024), (1024, QKVW)]

        def body(j, hT, hTk):
            import os
            if "p" in os.environ.get("ASKIP", ""):
                return
            raw, rawk = qkr.next()
            vt, vtk = vtr.next()
            for ci, (c0, c1) in enumerate(colsets):
                pb, pk = k.bank()
                wdt = c1 - c0

                def mm(e, pb=pb, c0=c0, c1=c1, wdt=wdt):
                    for kc in range(8):
                        e.matmul(pb[:, 0:wdt], lhsT=hT[:, kc, :], rhs=wq[:, kc, c0:c1], start=(kc == 0), stop=False)
                    return e.matmul(pb[:, 0:wdt], lhsT=C["onesb"][0:1, :], rhs=bq[0:1, c0:c1], start=False, stop=True)
                P.op("pe", mm, reads=[hTk, ("wq", 0), ("wq", 1), "bq", "onesb"], writes=[pk])
                if "E" in os.environ.get("ASKIP", ""):
                    continue
                if ci == 0:
                    P.op("act", lambda e, pb=pb, raw=raw: e.copy(out=raw[:, 0:512], in_=pb[:]), reads=[pk], writes=[rawk + "0"])
                elif ci == 1:
                    P.op("dve", lambda e, pb=pb, raw=raw: e.tensor_copy(out=raw[:, 512:1024], in_=pb[:]), reads=[pk], writes=[rawk + "1"])
                else:
                    P.op("dve", lambda e, pb=pb, raw=raw: e.tensor_copy(out=raw[:, D:D + KW], in_=pb[:, 0:KW]), reads=[pk], writes=[rawk + "2"])
                    P.op("dve", lambda e, pb=pb, vt=vt: e.tensor_copy(out=vt[:], in_=pb[:, KW:2 * KW]), reads=[pk], writes=[vtk])
                    if "V" not in os.environ.get("ASKIP", ""):
                        P.dma("sp", lambda e, vt=vt, j=j: e.dma_start(out=V[j * 128:(j + 1) * 128, :], in_=vt[:]), reads=[vtk], writes=[("V", j)])
            if "c" in os.environ.get("ASKIP", ""):
                return
            rawkeys = [rawk + "0", rawk + "1", rawk + "2"]
            import os
            SK = os.environ.get("ASKIP", "")
            if norm and "n" not in SK:
                sq, sqk = sqr.next()
                ss, ssk = ssr.next()
                qn_, qnk = qkn.next()
                P.op("act", lambda e, sq=sq, raw=raw: e.activation(out=sq[:], in_=raw[:], func=AF.Square), reads=rawkeys, writes=[sqk])
                P.op("dve", lambda e, sq=sq, ss=ss: e.tensor_reduce(out=ss[:, 0:NH], in_=sq[:].rearrange("p (h d) -> p h d", d=64),
                                                                  axis=mybir.AxisListType.X, op=ALU.add), reads=[sqk], writes=[ssk + "a"])
                P.op("dve", lambda e, ss=ss: e.tensor_scalar(out=ss[:, 0:NH], in0=ss[:, 0:NH], scalar1=1.0 / 64.0, scalar2=1e-6,
                                                             op0=ALU.mult, op1=ALU.add), reads=[ssk + "a"], writes=[ssk + "b"])
                P.op("act", lambda e, ss=ss: e.activation(out=ss[:, 0:NH], in_=ss[:, 0:NH], func=AF.Sqrt), reads=[ssk + "b"], writes=[ssk + "c"])
                P.op("dve", lambda e, ss=ss: e.reciprocal(out=ss[:, NH:2 * NH], in_=ss[:, 0:NH]), reads=[ssk + "c"], writes=[ssk])
                P.op("pool", lambda e, raw=raw: e.tensor_tensor(out=raw[:, 0:D], in0=raw[:, 0:D], in1=gq[:], op=ALU.mult),
                     reads=rawkeys + ["gq", sqk], writes=[rawk + "g"])
                P.op("pool", lambda e, raw=raw: e.tensor_tensor(out=raw[:, D:D + KW], in0=raw[:, D:D + KW], in1=gk[:], op=ALU.mult),
                     reads=rawkeys + ["gk", sqk], writes=[rawk + "h"])
                hkeys = []
                for h in range(NH):
                    eng = "dve" if h % 2 == 0 else "pool"
                    P.op(eng, lambda e, h=h, raw=raw, qn_=qn_, ss=ss: e.tensor_scalar(
                        out=qn_[:, h * 64:(h + 1) * 64], in0=raw[:, h * 64:(h + 1) * 64], scalar1=ss[:, NH + h:NH + h + 1], scalar2=None,
                        op0=ALU.mult), reads=[rawk + "g", rawk + "h", ssk], writes=[(qnk, h)])
                    hkeys.append((qnk, h))
                src, srckeys = qn_, hkeys
            else:
                src, srckeys = raw, rawkeys
            qr, qrk = qrr.next()
            if j < 128 and "r" not in SK:
                cs, csk = csr.next()
                P.dma("sp", lambda e, cs=cs, j=j: e.dma_start(out=cs[:], in_=rope_in[:, j * 128:(j + 1) * 128, :].rearrange("t p f -> p t f")),
                      writes=[csk])
                sv = src[:].rearrange("p (i two) -> p i two", two=2)
                ov = qr[:].rearrange("p (i two) -> p i two", two=2)
                segs = [(0, 512, 0), (512, HP, 0)]
                ta, tak = tmr.next()
                tb, tbk = tmr.next()
                tc_, tck = tmr.next()
                td, tdk = tmr.next()
                for (p0, p1, t0) in segs:
                    n = p1 - p0
                    sfx = str(p0)
                    P.op("dve", lambda e, p0=p0, p1=p1, t0=t0, n=n: e.tensor_tensor(out=ta[:, p0:p1], in0=sv[:, p0:p1, 0], in1=cs[:, 0, t0:t0 + n], op=ALU.mult),
                         reads=srckeys + [csk], writes=[tak + sfx])
                    P.op("pool", lambda e, p0=p0, p1=p1, t0=t0, n=n: e.tensor_tensor(out=tb[:, p0:p1], in0=sv[:, p0:p1, 1], in1=cs[:, 1, t0:t0 + n], op=ALU.mult),
                         reads=srckeys + [csk], writes=[tbk + sfx])
                    P.op("pool", lambda e, p0=p0, p1=p1, t0=t0, n=n: e.tensor_tensor(out=tc_[:, p0:p1], in0=sv[:, p0:p1, 0], in1=cs[:, 1, t0:t0 + n], op=ALU.mult),
                         reads=srckeys + [csk], writes=[tck + sfx])
                    P.op("dve", lambda e, p0=p0, p1=p1, t0=t0, n=n: e.tensor_tensor(out=td[:, p0:p1], in0=sv[:, p0:p1, 1], in1=cs[:, 0, t0:t0 + n], op=ALU.mult),
                         reads=srckeys + [csk], writes=[tdk + sfx])
                    P.op("dve", lambda e, p0=p0, p1=p1: e.tensor_tensor(out=ov[:, p0:p1, 0], in0=ta[:, p0:p1], in1=tb[:, p0:p1], op=ALU.subtract),
                         reads=[tak + sfx, tbk + sfx], writes=[qrk + "a" + sfx])
                    P.op("pool", lambda e, p0=p0, p1=p1: e.tensor_tensor(out=ov[:, p0:p1, 1], in0=tc_[:, p0:p1], in1=td[:, p0:p1], op=ALU.add),
                         reads=[tck + sfx, tdk + sfx], writes=[qrk + "b" + sfx])
                qrkeys = [qrk + a + str(p0) for a in "ab" for (p0, _, _) in segs]
            else:
                P.op("act", lambda e: e.copy(out=qr[:], in_=src[:]), reads=srckeys, writes=[qrk])
                qrkeys = [qrk]
            if "q" not in SK:
                P.dma("sp", lambda e, j=j: e.dma_start(out=Q[j * 128:(j + 1) * 128, :], in_=qr[:, 0:D]), reads=qrkeys, writes=[("Q", j)])
            if "k" in SK:
                return
            kTt, kTk = ktr.next()

            def trk(e):
                for hk in range(nkv):
                    ins = e.transpose(out=tpb[0:64, hk * 128:(hk + 1) * 128], in_=qr[:, D + hk * 64:D + (hk + 1) * 64], identity=ident[:])
                return ins
            P.op("pe", trk, reads=qrkeys + ["ident"], writes=["tpb"])
            P.op("act", lambda e: e.copy(out=kTt[:], in_=tpb[0:64, 0:nkv * 128]), reads=["tpb"], writes=[kTk])
            P.dma("sp", lambda e, j=j: e.dma_start(out=KT[:, :, j * 128:(j + 1) * 128].rearrange("h d t -> d h t"),
                                                 in_=kTt[:].rearrange("d (h t) -> d h t", h=nkv)), reads=[kTk], writes=[("KT", j)])
        stage_a(k, li, C, body)
    KTkeys = [("KT", j) for j in range(NSUB)]
    Vkeys = [("V", j) for j in range(NSUB)]
    if checkpoint(5):
        return None

    with k.sub():
        kTs = k.sb("kTs", [64, NTOK], BF16)
        vext = k.sb("vext", [128, NSUB, 65], BF16)
        P.op("pool", lambda e: e.memset(vext[:, :, 64:65], 1.0), writes=["vones"])
        if windowed:
            masks = k.sb("wmask", [128, 2, 512], BF16)
            P.dma("pool", lambda e: e.dma_start(out=masks[:], in_=mask_in.rearrange("t p f -> p t f")), writes=["wmask"])
        if sink:
            es = k.sb("es", [65, 2048])
            P.dma("sp", lambda e: e.dma_start(out=es[64:65, :], in_=sink_in), writes=["es0"])
            P.op("act", lambda e: e.activation(out=es[64:65, :], in_=es[64:65, :], func=AF.Exp), reads=["es0"], writes=["es"])
        GW = grp * 64
        qtr = Ring(k, "qt", 2, [128, GW], BF16)
        qTr = Ring(k, "qT", 2, [64, 512], BF16)
        ptr_ = Ring(k, "pt", 3, [128, 512], BF16)
        rdr = Ring(k, "rd", 2, [65, 512], F32)
        bcr = Ring(k, "bcs", 2, [64, 512], F32)
        otr = Ring(k, "ot", 2, [64, 512], BF16)
        qtiles = list(range(128)) + ([128, 129] if with_ctx else [])
        import os
        if "QT" in os.environ:
            qtiles = qtiles[:int(os.environ["QT"])]
        Vv = V.rearrange("(t p) c -> p t c", p=128)
        for hk in range(nkv):
            for q4 in range(5):
                t0, t1 = q4 * 26 * 128, (q4 + 1) * 26 * 128
                P.dma("sp", lambda e, hk=hk, t0=t0, t1=t1: e.dma_start(out=kTs[:, t0:t1], in_=KT[hk, :, t0:t1]), reads=KTkeys, writes=[("kTs", q4)])
                P.dma("sp", lambda e, hk=hk, q4=q4: e.dma_start(out=vext[:, q4 * 26:(q4 + 1) * 26, 0:64], in_=Vv[:, q4 * 26:(q4 + 1) * 26, hk * 64:(hk + 1) * 64]),
                      reads=Vkeys, writes=[("vext", q4)])
            kvkeys = [("kTs", q4) for q4 in range(5)] + [("vext", q4) for q4 in range(5)] + ["vones"]
            for i in qtiles:
                qt, qtk = qtr.next()
                P.dma("sp", lambda e, qt=qt, i=i, hk=hk: e.dma_start(out=qt[:], in_=Q[i * 128:(i + 1) * 128, hk * GW:(hk + 1) * GW]),
                      reads=[("Q", i)], writes=[qtk])
                if i >= 128:
                    kts = [(128, None), (129, None)]
                elif windowed:
                    kts = ([(i - 1, 0)] if i > 0 else []) + [(i, None)] + ([(i + 1, 1)] if i < 127 else []) + [(128, None), (129, None)]
                else:
                    kts = [(t, None) for t in range(NSUB)]
                for gh in range(grp // 4):
                    h0 = hk * grp + gh * 4
                    qT, qTk = qTr.next()

                    def trq(e, qt=qt, gh=gh):
                        for g in range(4):
                            ins = e.transpose(out=tpb[0:64, g * 128:(g + 1) * 128], in_=qt[:, (gh * 4 + g) * 64:(gh * 4 + g + 1) * 64], identity=ident[:])
                        return ins
                    P.op("pe", trq, reads=[qtk, "ident"], writes=["tpb"])
                    P.op("act", lambda e, qT=qT: e.copy(out=qT[:], in_=tpb[0:64, 0:512]), reads=["tpb"], writes=[qTk])
                    acc, acck = k.accs.next()
                    for idx, (kt, mk_) in enumerate(kts):
                        sbk, sk_ = k.bank()
                        P.op("pe", lambda e, sbk=sbk, kt=kt, qT=qT: e.matmul(sbk[:], lhsT=kTs[:, kt * 128:(kt + 1) * 128], rhs=qT[:], start=True, stop=True),
                             reads=kvkeys[0:5] + [qTk], writes=[sk_])
                        pt, ptk = ptr_.next()
                        P.op("act", lambda e, pt=pt, sbk=sbk: e.activation(out=pt[:], in_=sbk[:], func=AF.Exp, scale=0.125), reads=[sk_], writes=[ptk])
                        if mk_ is not None:
                            P.op("pool", lambda e, pt=pt, mk_=mk_: e.tensor_tensor(out=pt[:], in0=pt[:], in1=masks[:, mk_, :], op=ALU.mult),
                                 reads=[ptk, "wmask"], writes=[ptk])
                        P.op("pe", lambda e, acc=acc, kt=kt, pt=pt, idx=idx, n=len(kts): e.matmul(
                            acc[0:65, :], lhsT=vext[:, kt, :], rhs=pt[:], start=(idx == 0), stop=(idx == n - 1)),
                            reads=kvkeys[5:] + [ptk], writes=[acck])
                    rd, rdk = rdr.next()
                    if sink:
                        P.op("dve", lambda e, rd=rd, acc=acc, h0=h0: e.tensor_tensor(out=rd[64:65, :], in0=acc[64:65, :], in1=es[64:65, h0 * 128:(h0 + 4) * 128], op=ALU.add),
                             reads=[acck, "es"], writes=[rdk + "s"])
                        P.op("dve", lambda e, rd=rd: e.reciprocal(out=rd[64:65, :], in_=rd[64:65, :]), reads=[rdk + "s"], writes=[rdk])
                    else:
                        P.op("dve", lambda e, rd=rd, acc=acc: e.reciprocal(out=rd[64:65, :], in_=acc[64:65, :]), reads=[acck], writes=[rdk])
                    bb, bbk = k.bank()
                    P.op("pe", lambda e, bb=bb, rd=rd: e.matmul(bb[0:64, :], lhsT=C["ones32"][64:65, 0:64], rhs=rd[64:65, :], start=True, stop=True),
                         reads=[rdk, "ones32"], writes=[bbk])
                    bcs, bck = bcr.next()
                    P.op("act", lambda e, bcs=bcs, bb=bb: e.copy(out=bcs[:], in_=bb[0:64, :]), reads=[bbk], writes=[bck])
                    ot, otk = otr.next()
                    P.op("dve", lambda e, ot=ot, acc=acc, bcs=bcs: e.tensor_tensor(out=ot[:], in0=acc[0:64, :], in1=bcs[:], op=ALU.mult),
                         reads=[acck, bck], writes=[otk])
                    P.dma("sp", lambda e, ot=ot, i=i, h0=h0: e.dma_start(out=OT[i][:, h0:h0 + 4, :], in_=ot[:].rearrange("d (g t) -> d g t", g=4)),
                          reads=[otk], writes=[("OT", i, h0)])

    if checkpoint(6):
        return None
    wo = k.sb("wo", [64, 16, D], BF16)
    for hh in range(2):
        P.dma("pool", lambda e, hh=hh: e.dma_start(out=wo[:, hh * 8:(hh + 1) * 8, :],
                                                  in_=wo_in[hh * 512:(hh + 1) * 512, :].rearrange("(h p) n -> p h n", p=64)), writes=[("wo", hh)])
    bo = k.sb("bo", [1, D], BF16)
    P.dma("pool", lambda e: e.dma_start(out=bo[:], in_=bo_in), writes=["bo"])
    state = {}

    def y_src(j):
        if "r" not in state:
            state["r"] = Ring(k, "oTt", 2, [64, 16, 128], BF16)
        oT, oTk = state["r"].next()
        P.dma("sp", lambda e: e.dma_start(out=oT[:], in_=OT[j]), reads=[("OT", j, h0) for h0 in range(0, 16, 4)], writes=[oTk])
        res = []
        for half in range(2):
            pb, pk = k.bank()

            def mm(e, pb=pb, half=half):
                for h in range(16):
                    e.matmul(pb[:], lhsT=oT[:, h, :], rhs=wo[:, h, half * 512:(half + 1) * 512], start=(h == 0), stop=False)
                return e.matmul(pb[:], lhsT=C["onesb"][0:1, :], rhs=bo[0:1, half * 512:(half + 1) * 512], start=False, stop=True)
            P.op("pe", mm, reads=[oTk, ("wo", 0), ("wo", 1), "bo", "onesb"], writes=[pk])
            res.append((pb, pk))
        return res
    return y_src


def gmlp_front(k, li, C):
    nc, P = k.nc, k.P
    win_in = k.din(f"gm_win{li}", [D, 4096])
    bin_in = k.din(f"gm_bin{li}", [1, 4096])
    vng_in = k.din(f"gm_vng{li}", [1, 2048])
    vnb_in = k.din(f"gm_vnb{li}", [1, 2048])
    wsT_in = k.din(f"gm_wsT{li}", [8, 128, 128])
    bs_in = k.din(f"gm_bs{li}", [1, 1024])
    wo_in = k.din(f"gm_wo{li}", [2048, D])
    bo_in = k.din(f"gm_bo{li}", [1, D])
    UVT = k.dscr(f"gm_UVT{li}", [NSUB, 128, 16, 128], BF16)
    with k.sub():
        win = k.sb("win", [128, 8, 4096], BF16)
        for kc in range(8):
            P.dma("pool", lambda e, kc=kc: e.dma_start(out=win[:, kc, :], in_=win_in[kc * 128:(kc + 1) * 128, :]), writes=[("win", kc)])
        winkeys = [("win", kc) for kc in range(8)]
        binT = k.sb("binT", [128, 16])
        P.dma("sp", lambda e: e.dma_start(out=binT[:], in_=bin_in[0, 0:2048].rearrange("(ct p) -> p ct", p=128), allow_slow_non_contiguous=True),
              writes=["binT"])
        bvb = k.sb("bvb", [1, 2048], BF16)
        P.dma("pool", lambda e: e.dma_start(out=bvb[:], in_=bin_in[:, 2048:4096]), writes=["bvb"])
        vng = k.sb("vng", [128, 2048])
        P.dma("sp", lambda e: e.dma_start(out=vng[:], in_=vng_in.to_broadcast([128, 2048])), writes=["vng"])
        vnb = k.sb("vnb", [128, 2048])
        P.dma("sp", lambda e: e.dma_start(out=vnb[:], in_=vnb_in.to_broadcast([128, 2048])), writes=["vnb"])
        wsT = k.sb("wsT", [128, 8, 128], BF16)
        P.dma("pool", lambda e: e.dma_start(out=wsT[:], in_=wsT_in.rearrange("h q p -> q h p")), writes=["wsT"])
        bs32 = k.sb("bs32", [1, 1024])
        P.dma("sp", lambda e: e.dma_start(out=bs32[:], in_=bs_in), writes=["bs32"])
        bsh = k.sb("bsh", [1, 1024], BF16)
        bsl = k.sb("bsl", [1, 1024], BF16)
        bst = k.sb("bst", [1, 1024])
        P.op("act", lambda e: e.copy(out=bsh[:], in_=bs32[:]), reads=["bs32"], writes=["bsh"])
        P.op("dve", lambda e: e.tensor_copy(out=bst[:], in_=bsh[:]), reads=["bsh"], writes=["bst"])
        P.op("dve", lambda e: e.tensor_tensor(out=bst[:], in0=bs32[:], in1=bst[:], op=ALU.subtract), reads=["bs32", "bst"], writes=["bst2"])
        P.op("act", lambda e: e.copy(out=bsl[:], in_=bst[:]), reads=["bst2"], writes=["bsl"])
        uTr = Ring(k, "uT", 2, [128, 16, 128], BF16)
        vr = Ring(k, "vv", 2, [128, 2048], F32)
        vnr = Ring(k, "vn16", 2, [128, 2048], BF16)
        uvr = Ring(k, "uvT", 2, [128, 16, 128], BF16)
        stv = Ring(k, "stv", 2, [128, 24], F32)
        mvv = Ring(k, "mvv", 2, [128, 4], F32)

        def body(j, hT, hTk):
            uT, uTk = uTr.next()
            for cb in range(4):
                pb, pk = k.bank()

                def mmu(e, pb=pb, cb=cb):
                    for q in range(4):
                        ct = cb * 4 + q
                        for kc in range(8):
                            ins = e.matmul(pb[:, q * 128:(q + 1) * 128], lhsT=win[:, kc, ct * 128:(ct + 1) * 128], rhs=hT[:, kc, :],
                                           start=(kc == 0), stop=(kc == 7))
                    return ins
                P.op("pe", mmu, reads=[hTk] + winkeys, writes=[pk])
                for q in range(4):
                    ct = cb * 4 + q
                    P.op("act", lambda e, pb=pb, q=q, ct=ct: e.activation(out=uT[:, ct, :], in_=pb[:, q * 128:(q + 1) * 128], func=AF.Gelu,
                                                                       bias=binT[:, ct:ct + 1], scale=1.0), reads=[pk, "binT"], writes=[(uTk, ct)])
            v, vk = vr.next()
            for cb in range(4):
                pb, pk = k.bank()

                def mmv(e, pb=pb, cb=cb):
                    for kc in range(8):
                        e.matmul(pb[:], lhsT=hT[:, kc, :], rhs=win[:, kc, 2048 + cb * 512:2048 + (cb + 1) * 512], start=(kc == 0), stop=False)
                    return e.matmul(pb[:], lhsT=C["onesb"][0:1, :], rhs=bvb[0:1, cb * 512:(cb + 1) * 512], start=False, stop=True)
                P.op("pe", mmv, reads=[hTk, "bvb", "onesb"] + winkeys, writes=[pk])
                P.op("act", lambda e, pb=pb, cb=cb: e.activation(out=v[:, cb * 512:(cb + 1) * 512], in_=pb[:], func=AF.Gelu), reads=[pk], writes=[(vk, cb)])
            st, stk = stv.next()
            mv, mvk = mvv.next()
            for cb in range(4):
                P.op("dve", lambda e, cb=cb: e.bn_stats(out=st[:, cb * 6:(cb + 1) * 6], in_=v[:, cb * 512:(cb + 1) * 512]), reads=[(vk, cb)], writes=[(stk, cb)])
            P.op("dve", lambda e: e.bn_aggr(out=mv[:, 0:2], in_=st[:, 0:24]), reads=[(stk, cb) for cb in range(4)], writes=[mvk])
            P.op("act", lambda e: e.activation(out=mv[:, 2:3], in_=mv[:, 1:2], func=AF.Sqrt, bias=C["epsc"][:, 0:1], scale=1.0), reads=[mvk, "epsc"], writes=[mvk + "s"])
            P.op("dve", lambda e: e.reciprocal(out=mv[:, 3:4], in_=mv[:, 2:3]), reads=[mvk + "s"], writes=[mvk + "r"])
            P.op("dve", lambda e: e.tensor_scalar(out=v[:], in0=v[:], scalar1=mv[:, 0:1], scalar2=mv[:, 3:4], op0=ALU.subtract, op1=ALU.mult),
                 reads=[(vk, cb) for cb in range(4)] + [mvk, mvk + "r"], writes=[vk + "n"])
            P.op("pool", lambda e: e.tensor_tensor(out=v[:], in0=v[:], in1=vng[:], op=ALU.mult), reads=[vk + "n", "vng"], writes=[vk + "g"])
            vn, vnk = vnr.next()
            P.op("pool", lambda e: e.tensor_tensor(out=vn[:], in0=v[:], in1=vnb[:], op=ALU.add), reads=[vk + "g", "vnb"], writes=[vnk])
            uv, uvk = uvr.next()
            for cb in range(4):
                pb, pk = k.bank()

                def mms(e, pb=pb, cb=cb):
                    for q in range(4):
                        ct = cb * 4 + q
                        hg = ct // 2
                        e.matmul(pb[:, q * 128:(q + 1) * 128], lhsT=vn[:, ct * 128:(ct + 1) * 128], rhs=wsT[:, hg, :], start=True, stop=False)
                        e.matmul(pb[:, q * 128:(q + 1) * 128], lhsT=C["onesb"][0:1, :], rhs=bsh[0:1, hg * 128:(hg + 1) * 128], start=False, stop=False)
                        ins = e.matmul(pb[:, q * 128:(q + 1) * 128], lhsT=C["onesb"][0:1, :], rhs=bsl[0:1, hg * 128:(hg + 1) * 128], start=False, stop=True)
                    return ins
                P.op("pe", mms, reads=[vnk, "wsT", "bsh", "bsl", "onesb"], writes=[pk])
                P.op("dve", lambda e, pb=pb, cb=cb: e.tensor_tensor(out=uv[:, cb * 4:(cb + 1) * 4, :].rearrange("p a b -> p (a b)"), in0=pb[:],
                                                                 in1=uT[:, cb * 4:(cb + 1) * 4, :].rearrange("p a b -> p (a b)"), op=ALU.mult),
                     reads=[pk] + [(uTk, cb * 4 + q) for q in range(4)], writes=[(uvk, cb)])
            P.dma("sp", lambda e, j=j: e.dma_start(out=UVT[j], in_=uv[:]), reads=[(uvk, cb) for cb in range(4)], writes=[("UVT", j)])
        stage_a(k, li, C, body)

    wo = k.sb("gwo", [128, 16, D], BF16)
    for hh in range(4):
        P.dma("pool", lambda e, hh=hh: e.dma_start(out=wo[:, hh * 4:(hh + 1) * 4, :],
                                                  in_=wo_in[hh * 512:(hh + 1) * 512, :].rearrange("(c p) n -> p c n", p=128)), writes=[("gwo", hh)])
    bo = k.sb("gbo", [1, D], BF16)
    P.dma("pool", lambda e: e.dma_start(out=bo[:], in_=bo_in), writes=["gbo"])
    state = {}

    def y_src(j):
        if "r" not in state:
            state["r"] = Ring(k, "uvl", 2, [128, 16, 128], BF16)
        uv, uvk = state["r"].next()
        P.dma("sp", lambda e: e.dma_start(out=uv[:], in_=UVT[j]), reads=[("UVT", j)], writes=[uvk])
        res = []
        for half in range(2):
            pb, pk = k.bank()

            def mm(e, pb=pb, half=half):
                for ct in range(16):
                    e.matmul(pb[:], lhsT=uv[:, ct, :], rhs=wo[:, ct, half * 512:(half + 1) * 512], start=(ct == 0), stop=False)
                return e.matmul(pb[:], lhsT=C["onesb"][0:1, :], rhs=bo[0:1, half * 512:(half + 1) * 512], start=False, stop=True)
            P.op("pe", mm, reads=[uvk, "gbo", "onesb"] + [("gwo", hh) for hh in range(4)], writes=[pk])
            res.append((pb, pk))
        return res
    return y_src
```
